# Optimizing a Trainium2 kernel written in Bass

```python
import jax, jax.numpy as jnp
from jax import lax
import numpy as np


D_MODEL = 1024
BATCH = 8
SEQ = 4096
DEPTH = 4

HEAD_DIM = 64
FOX_HEADS = 4
NSA_HEADS = 8
NSA_KV_GROUPS = 2
DIL_HEADS = 4
MIX_WIDTH = (FOX_HEADS + NSA_HEADS + DIL_HEADS) * HEAD_DIM
DIL_PATTERNS = ((128, 1), (512, 4), (2048, 16))
ROPE_THETA = 500000.0
ROPE_DIM = HEAD_DIM // 4
Q_BLOCK = 128
CMP_BLOCK = 32
CMP_STRIDE = 16
CMP_HIDDEN = 4 * HEAD_DIM
SEL_BLOCK = 64
SEL_TOPK = 16
NSA_WINDOW = 512
D_FF = 2816
CONV_WIDTH = 3
RMS_EPS = 1e-6
NEG_INF = -1e30
FORCE_SCORE = 1e9
ATTN_SCALE = HEAD_DIM ** -0.5
KV_W = NSA_KV_GROUPS * HEAD_DIM
IN_SPLITS = (FOX_HEADS * HEAD_DIM,) * 3 + (FOX_HEADS,) + (NSA_HEADS * HEAD_DIM,) + (KV_W,) * 6 + (3 * NSA_HEADS,) + (DIL_HEADS * HEAD_DIM,) * 3
N_IN = sum(IN_SPLITS)

kernel_name = 'hybrid_fox_nsa_dilated_block'


def rms_norm(x, g):
    xf = x.astype(jnp.float32)
    y = xf * lax.rsqrt(jnp.mean(xf * xf, axis=-1, keepdims=True) + RMS_EPS)
    return (y * g.astype(jnp.float32)).astype(x.dtype)


def rope_tables(positions):
    half = ROPE_DIM // 2
    inv_freq = ROPE_THETA ** (-2.0 * jnp.arange(half, dtype=jnp.float32) / ROPE_DIM)
    ang = positions.astype(jnp.float32)[..., None] * inv_freq
    return jnp.cos(ang)[:, :, None, :], jnp.sin(ang)[:, :, None, :]


def partial_rope(x, cos, sin):
    half = ROPE_DIM // 2
    cos = cos.astype(x.dtype)
    sin = sin.astype(x.dtype)
    x1 = x[..., :half]
    x2 = x[..., half:ROPE_DIM]
    return jnp.concatenate([x1 * cos - x2 * sin, x2 * cos + x1 * sin, x[..., ROPE_DIM:]], axis=-1)


def masked_softmax(scores, mask):
    s = jnp.where(mask, scores, NEG_INF)
    m = jnp.max(s, axis=-1, keepdims=True)
    e = jnp.where(mask, jnp.exp(s - m), 0.0)
    den = jnp.maximum(jnp.sum(e, axis=-1, keepdims=True), 1e-30)
    return e / den, m + jnp.log(den)


def sweep_query_blocks(fn, seq):
    out = lax.map(fn, jnp.arange(seq // Q_BLOCK, dtype=jnp.int32) * Q_BLOCK)
    out = jnp.moveaxis(out, 0, 1)
    return out.reshape((out.shape[0], seq) + out.shape[3:])


def fox_attention(q, k, v, log_f):
    S = q.shape[1]
    c = jnp.cumsum(log_f, axis=1)
    c_k = jnp.transpose(c, (0, 2, 1))[:, :, None, :]
    kpos = jnp.arange(S)

    def block(start):
        qb = lax.dynamic_slice_in_dim(q, start, Q_BLOCK, axis=1)
        cb = lax.dynamic_slice_in_dim(c, start, Q_BLOCK, axis=1)
        qpos = start + jnp.arange(Q_BLOCK)
        s = jnp.einsum('bqhd,bkhd->bhqk', qb, k, preferred_element_type=jnp.float32) * ATTN_SCALE
        s = s + jnp.transpose(cb, (0, 2, 1))[..., None] - c_k
        p, _ = masked_softmax(s, kpos[None, :] <= qpos[:, None])
        return jnp.einsum('bhqk,bkhd->bqhd', p.astype(v.dtype), v)

    return sweep_query_blocks(block, S)


def dilated_attention(q, k, v):
    S = q.shape[1]

    def block(start):
        qb = lax.dynamic_slice_in_dim(q, start, Q_BLOCK, axis=1)
        qpos = start + jnp.arange(Q_BLOCK)
        outs, lses = [], []
        for window, dilation in DIL_PATTERNS:
            offs = jnp.arange(window // dilation + 1) * dilation
            idx = qpos[:, None] - offs[None, :]
            valid = idx >= 0
            idx = jnp.maximum(idx, 0)
            kg = jnp.take(k, idx, axis=1)
            vg = jnp.take(v, idx, axis=1)
            s = jnp.einsum('bqhd,bqjhd->bhqj', qb, kg, preferred_element_type=jnp.float32) * ATTN_SCALE
            p, lse = masked_softmax(s, valid[None, None])
            outs.append(jnp.einsum('bhqj,bqjhd->bqhd', p.astype(v.dtype), vg))
            lses.append(lse[..., 0])
        w = jax.nn.softmax(jnp.stack(lses, 0), axis=0)
        w = jnp.transpose(w, (0, 1, 3, 2))[..., None].astype(v.dtype)
        return jnp.sum(w * jnp.stack(outs, 0), axis=0)

    return sweep_query_blocks(block, S)


def nsa_compress(kv, pos_emb, w1, w2):
    S = kv.shape[1]
    n_cmp = (S - CMP_BLOCK) // CMP_STRIDE + 1
    idx = jnp.arange(n_cmp)[:, None] * CMP_STRIDE + jnp.arange(CMP_BLOCK)[None, :]
    win = jnp.take(kv, idx, axis=1) + pos_emb[None, None, :, None, :]
    B, N, L, G, hd = win.shape
    win = jnp.transpose(win, (0, 1, 3, 2, 4)).reshape(B, N, G, L * hd)
    return jax.nn.gelu(win @ w1) @ w2


def nsa_attention(q, kc, vc, ks, vs, kw, vw, gates, cmp_pos_k, cmp_w1_k, cmp_w2_k, cmp_pos_v, cmp_w1_v, cmp_w2_v):
    B, S, H, hd = q.shape
    G = NSA_KV_GROUPS
    R = H // G
    k_cmp = nsa_compress(kc, cmp_pos_k, cmp_w1_k, cmp_w2_k)
    v_cmp = nsa_compress(vc, cmp_pos_v, cmp_w1_v, cmp_w2_v)
    n_cmp = k_cmp.shape[1]
    cmp_start = jnp.arange(n_cmp) * CMP_STRIDE
    cmp_end = cmp_start + CMP_BLOCK - 1
    n_sel = S // SEL_BLOCK
    top_k = min(SEL_TOPK, n_sel)
    sel_start = jnp.arange(n_sel) * SEL_BLOCK
    overlap = ((cmp_start[:, None] < sel_start[None, :] + SEL_BLOCK) & (cmp_start[:, None] + CMP_BLOCK > sel_start[None, :])).astype(jnp.float32)
    ks_blk = jnp.transpose(ks.reshape(B, n_sel, SEL_BLOCK, G, hd), (0, 3, 1, 2, 4))
    vs_blk = jnp.transpose(vs.reshape(B, n_sel, SEL_BLOCK, G, hd), (0, 3, 1, 2, 4))
    pad = ((0, 0), (NSA_WINDOW, 0), (0, 0), (0, 0))
    kw_pad = jnp.pad(kw, pad)
    vw_pad = jnp.pad(vw, pad)
    gather_blocks = jax.vmap(jax.vmap(lambda blocks, idx: blocks[idx]))
    blk_id = jnp.arange(n_sel)[None, :]

    def block(start):
        qb = lax.dynamic_slice_in_dim(q, start, Q_BLOCK, axis=1).reshape(B, Q_BLOCK, G, R, hd)
        gb = lax.dynamic_slice_in_dim(gates, start, Q_BLOCK, axis=1).reshape(B, Q_BLOCK, G, R, 3)
        qpos = start + jnp.arange(Q_BLOCK)
        s_c = jnp.einsum('bqgrd,bngd->bgrqn', qb, k_cmp, preferred_element_type=jnp.float32) * ATTN_SCALE
        p_c, _ = masked_softmax(s_c, cmp_end[None, :] <= qpos[:, None])
        o_c = jnp.einsum('bgrqn,bngd->bqgrd', p_c.astype(v_cmp.dtype), v_cmp)
        imp = jnp.einsum('bgrqn,nj->bgqj', p_c, overlap)
        cur = (qpos // SEL_BLOCK)[:, None]
        forced = (blk_id == 0) | (blk_id == cur) | (blk_id == cur - 1)
        future = sel_start[None, :] > qpos[:, None]
        imp = jnp.where(future, NEG_INF, jnp.where(forced, FORCE_SCORE, imp))
        _, sel = lax.top_k(imp, top_k)
        k_sel = gather_blocks(ks_blk, sel).reshape(B, G, Q_BLOCK, top_k * SEL_BLOCK, hd)
        v_sel = gather_blocks(vs_blk, sel).reshape(B, G, Q_BLOCK, top_k * SEL_BLOCK, hd)
        kpos_sel = (sel[..., None] * SEL_BLOCK + jnp.arange(SEL_BLOCK)).reshape(B, G, Q_BLOCK, top_k * SEL_BLOCK)
        mask_s = (kpos_sel <= qpos[None, None, :, None])[:, :, None]
        s_s = jnp.einsum('bqgrd,bgqkd->bgrqk', qb, k_sel, preferred_element_type=jnp.float32) * ATTN_SCALE
        p_s, _ = masked_softmax(s_s, mask_s)
        o_s = jnp.einsum('bgrqk,bgqkd->bqgrd', p_s.astype(v_sel.dtype), v_sel)
        kwb = lax.dynamic_slice_in_dim(kw_pad, start, Q_BLOCK + NSA_WINDOW, axis=1)
        vwb = lax.dynamic_slice_in_dim(vw_pad, start, Q_BLOCK + NSA_WINDOW, axis=1)
        kpos_w = (start - NSA_WINDOW + jnp.arange(Q_BLOCK + NSA_WINDOW))[None, :]
        mask_w = (kpos_w <= qpos[:, None]) & (kpos_w > qpos[:, None] - NSA_WINDOW) & (kpos_w >= 0)
        s_w = jnp.einsum('bqgrd,bkgd->bgrqk', qb, kwb, preferred_element_type=jnp.float32) * ATTN_SCALE
        p_w, _ = masked_softmax(s_w, mask_w)
        o_w = jnp.einsum('bgrqk,bkgd->bqgrd', p_w.astype(vwb.dtype), vwb)
        o = gb[..., 0:1] * o_c + gb[..., 1:2] * o_s + gb[..., 2:3] * o_w
        return o.reshape(B, Q_BLOCK, H, hd)

    return sweep_query_blocks(block, S)


def hybrid_mixer(h, cos, sin, w_in, b_forget, b_nsa_gate, cmp_pos_k, cmp_w1_k, cmp_w2_k, cmp_pos_v, cmp_w1_v, cmp_w2_v, w_out):
    B, S, _ = h.shape
    proj = h @ w_in
    splits = np.cumsum(IN_SPLITS)[:-1].tolist()
    (fq, fk, fv, ff, nq, kc, vc, ks, vs, kw, vw, ng, dq, dk, dv) = jnp.split(proj, splits, axis=-1)

    def heads(t, n):
        return t.reshape(B, S, n, HEAD_DIM)

    log_f = jax.nn.log_sigmoid(ff.astype(jnp.float32) + b_forget.astype(jnp.float32))
    o_fox = fox_attention(heads(fq, FOX_HEADS), heads(fk, FOX_HEADS), heads(fv, FOX_HEADS), log_f)
    rope = lambda t, n: partial_rope(heads(t, n), cos, sin)
    gates = jax.nn.sigmoid(ng + b_nsa_gate).reshape(B, S, NSA_HEADS, 3)
    o_nsa = nsa_attention(rope(nq, NSA_HEADS), rope(kc, NSA_KV_GROUPS), heads(vc, NSA_KV_GROUPS),
                          rope(ks, NSA_KV_GROUPS), heads(vs, NSA_KV_GROUPS),
                          rope(kw, NSA_KV_GROUPS), heads(vw, NSA_KV_GROUPS), gates,
                          cmp_pos_k, cmp_w1_k, cmp_w2_k, cmp_pos_v, cmp_w1_v, cmp_w2_v)
    o_dil = dilated_attention(rope(dq, DIL_HEADS), rope(dk, DIL_HEADS), heads(dv, DIL_HEADS))
    mix = jnp.concatenate([o_fox.reshape(B, S, -1), o_nsa.reshape(B, S, -1), o_dil.reshape(B, S, -1)], axis=-1)
    return mix @ w_out


def conv_ffn(h, w_up, conv_w, conv_b, w_down):
    S = h.shape[1]
    u = h @ w_up
    u_pad = jnp.pad(u, ((0, 0), (CONV_WIDTH - 1, 0), (0, 0)))
    c = conv_b + u_pad[:, 0:S] * conv_w[0]
    for j in range(1, CONV_WIDTH):
        c = c + u_pad[:, j:j + S] * conv_w[j]
    a, b = jnp.split(c, 2, axis=-1)
    return (jax.nn.silu(a) * b) @ w_down


def setup_inputs(seed: int = 0) -> dict:
    key = jax.random.key(seed)
    k = jax.random.split(key, 24)
    nrm = lambda kk, shape, scale: jax.random.normal(kk, shape, jnp.float32) * scale
    x = nrm(k[0], (BATCH, SEQ, D_MODEL), 1.0)
    offset = jax.random.randint(k[1], (BATCH, 1), 0, 1024, dtype=jnp.int32)
    positions = offset + jnp.arange(SEQ, dtype=jnp.int32)[None, :]
    return {
        'x': x,
        'positions': positions,
        'attn_pre_norm': 1.0 + nrm(k[2], (DEPTH, D_MODEL), 0.05),
        'attn_post_norm': 1.0 + nrm(k[3], (DEPTH, D_MODEL), 0.05),
        'ffn_pre_norm': 1.0 + nrm(k[4], (DEPTH, D_MODEL), 0.05),
        'ffn_post_norm': 1.0 + nrm(k[5], (DEPTH, D_MODEL), 0.05),
        'w_in': nrm(k[6], (DEPTH, D_MODEL, N_IN), D_MODEL ** -0.5),
        'b_forget': jax.random.uniform(k[7], (DEPTH, FOX_HEADS), jnp.float32, 1.0, 5.0),
        'b_nsa_gate': nrm(k[8], (DEPTH, 3 * NSA_HEADS), 0.01),
        'cmp_pos_k': nrm(k[9], (DEPTH, CMP_BLOCK, HEAD_DIM), 0.1),
        'cmp_w1_k': nrm(k[10], (DEPTH, CMP_BLOCK * HEAD_DIM, CMP_HIDDEN), (CMP_BLOCK * HEAD_DIM) ** -0.5),
        'cmp_w2_k': nrm(k[11], (DEPTH, CMP_HIDDEN, HEAD_DIM), CMP_HIDDEN ** -0.5),
        'cmp_pos_v': nrm(k[12], (DEPTH, CMP_BLOCK, HEAD_DIM), 0.1),
        'cmp_w1_v': nrm(k[13], (DEPTH, CMP_BLOCK * HEAD_DIM, CMP_HIDDEN), (CMP_BLOCK * HEAD_DIM) ** -0.5),
        'cmp_w2_v': nrm(k[14], (DEPTH, CMP_HIDDEN, HEAD_DIM), CMP_HIDDEN ** -0.5),
        'w_out': nrm(k[15], (DEPTH, MIX_WIDTH, D_MODEL), MIX_WIDTH ** -0.5),
        'w_up': nrm(k[16], (DEPTH, D_MODEL, 2 * D_FF), D_MODEL ** -0.5),
        'conv_w': nrm(k[17], (DEPTH, CONV_WIDTH, 2 * D_FF), CONV_WIDTH ** -0.5),
        'conv_b': nrm(k[18], (DEPTH, 2 * D_FF), 0.01),
        'w_down': nrm(k[19], (DEPTH, D_FF, D_MODEL), D_FF ** -0.5),
    }


def reference(x, positions, attn_pre_norm, attn_post_norm, ffn_pre_norm, ffn_post_norm, w_in, b_forget, b_nsa_gate,
              cmp_pos_k, cmp_w1_k, cmp_w2_k, cmp_pos_v, cmp_w1_v, cmp_w2_v, w_out, w_up, conv_w, conv_b, w_down):
    cos, sin = rope_tables(positions)
    for l in range(DEPTH):
        h = rms_norm(x, attn_pre_norm[l])
        mix = hybrid_mixer(h, cos, sin, w_in[l], b_forget[l], b_nsa_gate[l], cmp_pos_k[l], cmp_w1_k[l], cmp_w2_k[l],
                           cmp_pos_v[l], cmp_w1_v[l], cmp_w2_v[l], w_out[l])
        x = x + rms_norm(mix, attn_post_norm[l])
        h = rms_norm(x, ffn_pre_norm[l])
        x = x + rms_norm(conv_ffn(h, w_up[l], conv_w[l], conv_b[l], w_down[l]), ffn_post_norm[l])
    return x
```

```python
import math
from contextlib import ExitStack, contextmanager

import numpy as np
import ml_dtypes
import concourse.bass as bass
import concourse.mybir as mybir
from concourse.bass_utils import run_bass_kernel_spmd

F32 = mybir.dt.float32
BF16 = mybir.dt.bfloat16
I32 = mybir.dt.int32
AF = mybir.ActivationFunctionType
ALU = mybir.AluOpType

S = 4096
D = 1024
NB = 32
NQT = 8
DFF = 2816
DEPTH = 4
STAGE = 8
UID0 = 0
NTT_B = 8
SUBB = 9
NSUB = 9
NSA_I = 8
NSA_G = 2
NEG = -30000.0
IN_SPLITS = (256,) * 3 + (4,) + (512,) + (128,) * 6 + (24,) + (256,) * 3
T_FQ, T_FK, T_NQ, T_KC, T_KS, T_KW, T_DQ, T_DK, T_VC = 0, 4, 8, 16, 18, 20, 22, 26, 30
NTILES = 32
M_CAUSAL, M_WIN, M_DIL, M_CMP, NMASK = 0, 4, 8, 21, 26


def dil_mask_index(o):
    if o <= -13:
        return M_DIL + (o + 16)
    if o <= -5:
        return M_DIL + 4
    return M_DIL + 5 + (o + 4)


def _col_layout():
    off = np.cumsum((0,) + IN_SPLITS)
    fq, fk, fv, ff, nq, kc, vc, ks, vs, kw, vw, ng, dq, dk, dv = [int(v) for v in off[:15]]
    tiles = []
    tiles += [(fq + 64 * h, False) for h in range(4)]
    tiles += [(fk + 64 * h, False) for h in range(4)]
    tiles += [(nq + 64 * h, True) for h in range(8)]
    tiles += [(kc + 64 * g, True) for g in range(2)]
    tiles += [(ks + 64 * g, True) for g in range(2)]
    tiles += [(kw + 64 * g, True) for g in range(2)]
    tiles += [(dq + 64 * h, True) for h in range(4)]
    tiles += [(dk + 64 * h, True) for h in range(4)]
    tiles += [(vc + 64 * g, False) for g in range(2)]
    cols = []
    tinfo = []
    for c0, roped in tiles:
        start = len(cols)
        cols += list(range(c0, c0 + 64))
        if roped:
            cols += list(range(c0 + 8, c0 + 16)) + list(range(c0, c0 + 8))
        tinfo.append((start, 80 if roped else 64, roped))
    c_ff = len(cols)
    cols += list(range(ff, ff + 4))
    c_tm1 = len(cols)
    cols += list(range(fv, fv + 256)) + list(range(vs, vs + 128)) + list(range(vw, vw + 128))
    c_tm2 = len(cols)
    cols += list(range(dv, dv + 256)) + list(range(ng, ng + 24))
    return np.asarray(cols, np.int64), tinfo, c_ff, c_tm1, c_tm2


COLS, TINFO, C_FF, C_TM1, C_TM2 = _col_layout()
NC_EXT = len(COLS)


def _masks():
    k = np.arange(128)[:, None].astype(np.int64)
    q = np.arange(512)[None, :].astype(np.int64)
    m = np.zeros((NMASK, 128, 512), np.float32)
    for o in range(4):
        m[M_CAUSAL + o] = np.where(q - 128 * o - k >= 0, 0.0, NEG)
        m[M_WIN + o] = np.where(k + 128 * o - q > 0, 0.0, NEG)
    seen = {}
    for o in range(-16, 4):
        dl = q - k - 128 * o
        mult = ((dl >= 0) & (dl <= 128)).astype(np.int64) + ((dl >= 0) & (dl <= 512) & (dl % 4 == 0)) \
            + ((dl >= 0) & (dl <= 2048) & (dl % 16 == 0))
        val = np.where(mult > 0, 8.0 * np.log(np.maximum(mult, 1)), NEG).astype(np.float32)
        idx = dil_mask_index(o)
        if idx in seen:
            assert np.array_equal(seen[idx], val), o
        seen[idx] = val
        m[idx] = val
    for v in range(5):
        m[M_CMP + v] = np.where(16 * k + 31 - 512 * v <= q, 0.0, NEG)
    return m


MASKS = _masks()
MSKIP = (MASKS.reshape(NMASK, 128, 4, 128).max(axis=(1, 3)) <= NEG + 1)


def _consts():
    c = {}
    c["masks"] = np.ascontiguousarray(MASKS.transpose(1, 0, 2)).astype(ml_dtypes.bfloat16)
    E = np.zeros((128, S), np.float32)
    E[np.arange(S) // 64, np.arange(S)] = 1.0
    c["emat"] = E.astype(ml_dtypes.bfloat16)
    c["identb"] = np.eye(128, dtype=np.float32).astype(ml_dtypes.bfloat16)
    c["identf"] = np.eye(128, dtype=np.float32)
    n = (np.arange(256).reshape(2, 128).T)[:, :, None]
    j = np.arange(64)[None, None, :]
    ov = ((16 * n < 64 * j + 64) & (16 * n + 32 > 64 * j) & (n < 255)).astype(np.float32)
    c["ovt"] = ov.astype(ml_dtypes.bfloat16)
    p = np.arange(128)[:, None]
    jj = np.arange(127)[None, :] - 63
    cur = (p >= 64).astype(np.int64)
    forced = (jj == cur) | (jj == cur - 1)
    future = jj > cur
    keep = np.where(forced | future, 0.0, 1.0).astype(np.float32)
    add = np.where(future, -1e30, np.where(forced, 1e9, 0.0)).astype(np.float32)
    c["keepw"] = keep
    c["addw"] = add
    half = 8
    inv = (500000.0 ** (-2.0 * np.arange(half, dtype=np.float32) / 16.0)).astype(np.float32)
    rc = np.zeros((80, 2), np.float32)
    for base in (0, 64):
        for i in range(16):
            rc[base + i, 0] = inv[i % 8]
            rc[base + i, 1] = -1.0 if i < 8 else 1.0
    c["ropec"] = rc
    return c


CONSTS = _consts()


class Buf:
    __slots__ = ("name", "w", "r", "psum")

    def __init__(self, name, psum=False):
        self.name = name
        self.w = {}
        self.r = {}
        self.psum = psum


class V:
    __slots__ = ("ap", "buf")

    def __init__(self, ap, buf):
        self.ap = ap
        self.buf = buf

    def __getitem__(self, idx):
        return V(self.ap[idx], self.buf)

    def re(self, pat, **kw):
        return V(self.ap.rearrange(pat, **kw), self.buf)

    def bc(self, shape):
        return V(self.ap.to_broadcast(list(shape)), self.buf)


class Prog:
    ENG = ("pe", "act", "dve", "pool", "sp")
    NDMA = 8

    def __init__(self, nc, es):
        self.nc = nc
        self.es = es
        self.ops = {e: [] for e in self.ENG}
        self.cnt = {e: 0 for e in self.ENG}
        self.keysem = {e: es.enter_context(nc.semaphore("s_" + e)) for e in self.ENG}
        self.dsem = {}
        for q in ("sp", "pool"):
            self.dsem[q] = []
            for i in range(self.NDMA):
                sem = es.enter_context(nc.semaphore(f"d_{q}{i}"))
                self.keysem[("d", q, i)] = sem
                self.dsem[q].append([sem, 0])
        self.drr = {q: 0 for q in self.dsem}
        self.waited = {e: {} for e in self.ENG}
        self.pending = {e: {} for e in self.ENG}
        self.uid = UID0

    def _name(self, name):
        self.uid += 1
        return f"{name}_{self.uid}"

    def sb(self, name, shape, dt):
        t = self.es.enter_context(self.nc.sbuf_tensor(self._name(name), list(shape), dt))
        return V(t[:], Buf(name))

    def ps(self, name, shape, dt=F32):
        full = 512 if dt == F32 else 1024
        t = self.es.enter_context(self.nc.psum_tensor(self._name(name), [128, full], dt))
        shape = list(shape)
        n = int(np.prod(shape[1:]))
        assert n <= full
        ap = t[:][0:shape[0], 0:n]
        if len(shape) == 3:
            ap = ap.rearrange("p (a b) -> p a b", b=shape[2])
        elif len(shape) == 4:
            ap = ap.rearrange("p (a b c) -> p a b c", b=shape[2], c=shape[3])
        return V(ap, Buf(name, psum=True))

    def dram(self, name, shape, dt, kind="Internal"):
        t = self.nc.dram_tensor(name, list(shape), dt, kind=kind)
        return V(t.ap(), Buf(name))

    def barrier(self):
        snap = {e: self.cnt[e] for e in self.ENG if self.cnt[e] > 0}
        for q, slots in self.dsem.items():
            for i, (sem, val) in enumerate(slots):
                if val > 0:
                    snap[("d", q, i)] = val
        for e in self.ENG:
            p = self.pending[e]
            for k, v in snap.items():
                if p.get(k, 0) < v:
                    p[k] = v

    @contextmanager
    def scope(self):
        self.barrier()
        old = self.es
        with ExitStack() as es:
            self.es = es
            yield
            self.barrier()
        self.es = old

    def _deps(self, eng, reads, writes):
        deps = self.pending[eng]
        self.pending[eng] = {}

        def mer(d):
            for k, v in d.items():
                if deps.get(k, 0) < v:
                    deps[k] = v
        for v in reads:
            mer(v.buf.w)
            if v.buf.psum:
                mer({k: c for k, c in v.buf.r.items() if k != eng})
        for v in writes:
            mer(v.buf.w)
            mer(v.buf.r)
        w = self.waited[eng]
        out = []
        for k, v in deps.items():
            if k == "pe" and eng == "pe":
                continue
            if w.get(k, 0) < v:
                w[k] = v
                out.append((k, v))
        return out

    def _mark(self, key, val, reads, writes):
        for v in reads:
            if v.buf.r.get(key, 0) < val:
                v.buf.r[key] = val
        for v in writes:
            if v.buf.w.get(key, 0) < val:
                v.buf.w[key] = val

    def op(self, eng, fn, reads, writes):
        waits = self._deps(eng, reads, writes)
        self.cnt[eng] += 1
        c = self.cnt[eng]
        self.ops[eng].append((waits, fn, eng))
        self._mark(eng, c, reads, writes)

    def dma(self, q, out, in_, **kw):
        i = self.drr[q]
        slot = self.dsem[q][i]
        key = ("d", q, i)
        self.drr[q] = (i + 1) % self.NDMA
        waits = self._deps(q, [in_], [out])
        if slot[1] > 0 and self.waited[q].get(key, 0) < slot[1]:
            self.waited[q][key] = slot[1]
            waits.append((key, slot[1]))
        slot[1] += 16
        oap, iap = out.ap, in_.ap
        self.ops[q].append((waits, lambda e: e.dma_start(out=oap, in_=iap, **kw), key))
        self._mark(key, slot[1], [in_], [out])

    def wait_all(self, eng, vs):
        waits = self._deps(eng, vs, [])
        self.ops[eng].append((waits, None, None))

    def emit(self):
        nc = self.nc
        with nc.Block() as block:
            def run(e, name):
                for waits, fn, inc in self.ops[name]:
                    for k, v in waits:
                        e.wait_ge(self.keysem[k], v)
                    if fn is None:
                        continue
                    ins = fn(e)
                    ins.then_inc(self.keysem[inc], 16 if isinstance(inc, tuple) else 1)

            @block.tensor
            def _(e):
                run(e, "pe")

            @block.scalar
            def _(e):
                run(e, "act")

            @block.vector
            def _(e):
                run(e, "dve")

            @block.gpsimd
            def _(e):
                run(e, "pool")

            @block.sync
            def _(e):
                run(e, "sp")

    def mm(self, out, lhsT, rhs, start=True, stop=True):
        o, l, r = out.ap, lhsT.ap, rhs.ap
        self.op("pe", lambda e: e.matmul(o, l, r, start=start, stop=stop), [lhsT, rhs], [out])

    def tr(self, out, in_, ident):
        o, i, d = out.ap, in_.ap, ident.ap
        self.op("pe", lambda e: e.transpose(o, i, d), [in_, ident], [out])

    def act(self, out, in_, func, bias=None, scale=None, accum_out=None):
        kw = {}
        rd = [in_]
        wr = [out]
        if bias is not None:
            if isinstance(bias, V):
                kw["bias"] = bias.ap
                rd.append(bias)
            else:
                kw["bias"] = bias
        if scale is not None:
            if isinstance(scale, V):
                kw["scale"] = scale.ap
                rd.append(scale)
            else:
                kw["scale"] = scale
        if accum_out is not None:
            kw["accum_out"] = accum_out.ap
            wr.append(accum_out)
        o, i = out.ap, in_.ap
        self.op("act", lambda e: e.activation(o, i, func, **kw), rd, wr)

    def ts(self, eng, out, in0, s1, op0, s2=None, op1=None):
        rd = [in0]
        a1 = s1.ap if isinstance(s1, V) else s1
        a2 = s2.ap if isinstance(s2, V) else s2
        if isinstance(s1, V):
            rd.append(s1)
        if isinstance(s2, V):
            rd.append(s2)
        kw = {}
        if op1 is not None:
            kw["op1"] = op1
        o, i = out.ap, in0.ap
        self.op(eng, lambda e: e.tensor_scalar(o, i, a1, a2, op0, **kw), rd, [out])

    def tt(self, eng, out, in0, in1, op):
        o, a, b = out.ap, in0.ap, in1.ap
        self.op(eng, lambda e: e.tensor_tensor(o, a, b, op), [in0, in1], [out])

    def stt(self, out, in0, scalar, in1, op0, op1):
        rd = [in0, in1]
        s = scalar.ap if isinstance(scalar, V) else scalar
        if isinstance(scalar, V):
            rd.append(scalar)
        o, a, b = out.ap, in0.ap, in1.ap
        self.op("dve", lambda e: e.scalar_tensor_tensor(o, a, s, b, op0, op1), rd, [out])

    def copy(self, eng, out, in_):
        o, i = out.ap, in_.ap
        if eng == "act":
            self.op("act", lambda e: e.copy(o, i), [in_], [out])
        else:
            self.op(eng, lambda e: e.tensor_copy(o, i), [in_], [out])

    def memset(self, eng, out, val):
        o = out.ap
        self.op(eng, lambda e: e.memset(o, val), [], [out])

    def recip(self, out, in_):
        o, i = out.ap, in_.ap
        self.op("dve", lambda e: e.reciprocal(o, i), [in_], [out])


class Net:
    def __init__(self, nlayers, dbg=False):
        self.nl = nlayers
        self.dbg = dbg
        self.nc = bass.Bass("TRN2", target_bir_lowering=False)
        self.es = ExitStack()

    def build(self):
        nc = self.nc
        nl = self.nl
        with self.es:
            P = self.P = Prog(nc, self.es)
            ext = lambda n, s, d: P.dram(n, s, d, kind="ExternalInput")
            self.x_in = ext("x", [S, D], F32)
            self.pos = ext("pos", [1, S], I32)
            self.w_in = ext("w_in", [nl, D, NC_EXT], F32)
            small_dbg = self.dbg and STAGE < 6
            if small_dbg:
                self.w_out = P.dram("w_out", [nl, D, D], F32)
                self.w_up = P.dram("w_up", [nl, D, 2 * DFF], F32)
                self.w_down = P.dram("w_down", [nl, DFF, D], F32)
            else:
                self.w_out = ext("w_out", [nl, D, D], F32)
                self.w_up = ext("w_up", [nl, D, 2 * DFF], F32)
                self.w_down = ext("w_down", [nl, DFF, D], F32)
            self.norms = ext("norms", [nl, 4, D], F32)
            self.b_forget = ext("b_forget", [nl, 4, 1], F32)
            self.b_gate = ext("b_gate", [nl, 1, 24], F32)
            self.posT = ext("posT", [nl, 2, 64, 32], F32)
            self.w1 = ext("w1", [nl, 2, 64, 32, 256], F32)
            self.w2 = ext("w2", [nl, 2, 256, 64], F32)
            self.cw = ext("cw", [nl, 128, 44, 3], F32)
            self.cb = ext("cb", [nl, 128, 44], F32)
            self.c_masks = ext("masks", [128, NMASK, 512], BF16)
            self.c_emat = ext("emat", [128, S], BF16)
            self.c_identb = ext("identb", [128, 128], BF16)
            self.c_identf = ext("identf", [128, 128], F32)
            self.c_ovt = ext("ovt", [128, 2, 64], BF16)
            self.c_keepw = ext("keepw", [128, 127], F32)
            self.c_addw = ext("addw", [128, 127], F32)
            self.c_ropec = ext("ropec", [80, 2], F32)
            self.x_out = P.dram("y", [S, D] if not small_dbg else [128, 8], F32, kind="ExternalOutput")
            k = "ExternalOutput" if self.dbg else "Internal"
            k2 = "ExternalOutput" if (self.dbg and not small_dbg) else "Internal"
            self.QK = P.dram("qk_scr", [68, NTILES, S], BF16, kind=k)
            self.VA = P.dram("va_scr", [12, 128, NB, 65], BF16, kind=k)
            self.MIX = P.dram("mix_scr", [S, D], BF16, kind=k if STAGE >= 3 else "Internal")
            self.GT = P.dram("gt_scr", [22, 128, S], BF16, kind="Internal")
            self.TAB = P.dram("tab_scr", [2, 80, S], F32, kind=k)
            self.XA = P.dram("xa_scr", [S, D], F32, kind=k2)
            self.XB = P.dram("xb_scr", [S, D], F32, kind="Internal")

            self.identb = P.sb("identb", [128, 128], BF16)
            self.identf = P.sb("identf", [128, 128], F32)
            P.dma("sp", self.identb, self.c_identb)
            P.dma("sp", self.identf, self.c_identf)

            self.rope_tables()
            src = self.x_in
            for L in range(nl):
                last = L == nl - 1
                mid = self.XA
                dst = self.x_out if last else self.XB
                self.layer(L, src, mid, dst)
                src = dst
            if not small_dbg:
                P.wait_all("sp", [self.x_out])
            P.emit()
        return nc

    def rope_tables(self):
        P = self.P
        with P.scope():
            rc = P.sb("ropec", [80, 2], F32)
            P.dma("sp", rc, self.c_ropec)
            pi = P.sb("posi", [80, S], I32)
            P.dma("sp", pi, self.pos.bc([80, S]))
            ang = P.sb("ang", [80, S], F32)
            P.copy("dve", ang, pi)
            P.ts("dve", ang, ang, rc[:, 0:1], ALU.mult)
            tmp = P.sb("rtmp", [80, S], F32)
            ki = P.sb("rki", [80, S], I32)
            kf = P.sb("rkf", [80, S], F32)
            res = P.sb("rres", [80, S], F32)
            TWO_PI = 2.0 * math.pi
            C1 = 6.28125
            C2 = TWO_PI - C1
            for which in range(2):
                src = ang
                if which == 0:
                    P.ts("dve", tmp, ang, math.pi / 2.0, ALU.add)
                    src = tmp
                P.ts("dve", kf, src, 1.0 / TWO_PI, ALU.mult)
                P.copy("dve", ki, kf)
                P.copy("dve", kf, ki)
                P.stt(res, kf, -C1, src, ALU.mult, ALU.add)
                P.stt(res, kf, -C2, res, ALU.mult, ALU.add)
                P.ts("dve", kf, res, math.pi, ALU.is_gt)
                P.stt(res, kf, -TWO_PI, res, ALU.mult, ALU.add)
                P.ts("dve", kf, res, -math.pi, ALU.is_lt)
                P.stt(res, kf, TWO_PI, res, ALU.mult, ALU.add)
                P.ts("dve", res, res, math.pi, ALU.min, -math.pi, ALU.max)
                P.act(res, res, AF.Sin)
                if which == 1:
                    P.ts("dve", res, res, rc[:, 1:2], ALU.mult)
                P.dma("sp", self.TAB[which], res)

    def load_cast(self, dst, src, nfree_split):
        P = self.P
        n = dst.ap.shape[1]
        step = (n + nfree_split - 1) // nfree_split
        for a in range(0, n, step):
            b = min(n, a + step)
            P.dma("pool", dst[:, a:b], src[:, a:b], max_dma_last_dim=4096)

    def rms_rstd(self, ss, rstd, sd):
        P = self.P
        P.act(sd, ss, AF.Sqrt, bias=self.epsc, scale=1.0 / D)
        P.recip(rstd, sd)

    def norm_transpose(self, tt, xsrc, gbc, xts, hb, hT, ptr, junk, small):
        P = self.P
        for s in range(4):
            blk = 4 * tt + s
            xt = xts[blk % len(xts)]
            P.dma("sp", xt, xsrc[blk * 128:(blk + 1) * 128, :])
            ss, sd, rstd = small[blk % 2]
            P.act(junk, xt, AF.Square, accum_out=ss)
            self.rms_rstd(ss, rstd, sd)
            P.stt(hb[:, s, :], xt, rstd, gbc, ALU.mult, ALU.mult)
        for kc in range(8):
            pt = ptr[kc % 2]
            for s in range(4):
                P.tr(pt[:, s * 128:(s + 1) * 128], hb[:, s, kc * 128:(kc + 1) * 128], self.identb)
            P.copy("act" if kc % 2 == 0 else "dve", hT[:, kc, :], pt)

    def post_norm_residual(self, blk, py, gbc, xsrc, xdst, xts, xns, small, junk):
        P = self.P
        ss0, ss1, sd, rstd = small[blk % 2]
        P.act(junk[:, 0:512], py[0], AF.Square, accum_out=ss0)
        P.act(junk[:, 512:1024], py[1], AF.Square, accum_out=ss1)
        P.tt("dve", ss0, ss0, ss1, ALU.add)
        self.rms_rstd(ss0, rstd, sd)
        xt = xts[blk % len(xts)]
        P.dma("sp", xt, xsrc[blk * 128:(blk + 1) * 128, :])
        xn = xns[blk % len(xns)]
        for hf in range(2):
            sl = slice(hf * 512, (hf + 1) * 512)
            P.stt(xn[:, sl], py[hf], rstd, gbc[:, sl], ALU.mult, ALU.mult)
            P.tt("pool", xn[:, sl], xn[:, sl], xt[:, sl], ALU.add)
        P.dma("sp", xdst[blk * 128:(blk + 1) * 128, :], xn)

    def layer(self, L, xsrc, xmid, xdst):
        P = self.P
        with P.scope():
            self.epsc = P.sb("epsc", [128, 1], F32)
            P.memset("dve", self.epsc, 1e-6)
            self.onec = P.sb("onec", [128, 1], F32)
            P.memset("dve", self.onec, 1.0)
            with P.scope():
                gates = P.sb("gates", [128, NB, 24], F32)
                cs = P.sb("cs", [128, NB, 4], F32)
                with P.scope():
                    lnf = P.sb("lnf", [4, S], F32)
                    if STAGE >= 1:
                        self.phase_B(L, xsrc, lnf, gates)
                    if STAGE >= 2:
                        self.phase_B2(lnf, cs)
                if STAGE >= 3:
                    self.phase_C(L, gates, cs)
            if STAGE >= 6:
                self.phase_D(L, xsrc, xmid)
            if STAGE >= 7:
                self.phase_E1(L, xmid)
            if STAGE >= 8:
                self.phase_E2(L, xmid, xdst)

    def gain_bc(self, name, L, which):
        P = self.P
        g = P.sb(name, [128, D], F32)
        P.dma("sp", g, self.norms[L, which:which + 1, :].bc([128, D]))
        return g

    def phase_B(self, L, xsrc, lnf, gates):
        P = self.P
        with P.scope():
            wi = P.sb("wi", [128, 8, NC_EXT], BF16)
            wsrc = self.w_in[L].re("(kc p) n -> p kc n", p=128)
            for c0 in range(0, NC_EXT, 800):
                c1 = min(NC_EXT, c0 + 800)
                P.dma("pool", wi[:, :, c0:c1], wsrc[:, :, c0:c1])
            gbc = self.gain_bc("gbc", L, 0)
            cosTs = [P.sb(f"cosT{i}", [16, 512], F32) for i in range(2)]
            sinTs = [P.sb(f"sinT{i}", [16, 512], F32) for i in range(2)]
            negb = P.sb("negb", [4, 1], F32)
            P.dma("sp", negb, self.b_forget[L])
            P.ts("dve", negb, negb, -1.0, ALU.mult)
            bg = P.sb("bg", [128, 24], F32)
            P.dma("sp", bg, self.b_gate[L].bc([128, 24]))
            xts = [P.sb(f"xt{i}", [128, D], F32) for i in range(2)]
            junk = P.sb("junk", [128, D], BF16)
            small = [tuple(P.sb(f"sm{i}{j}", [128, 1], F32) for j in range(3)) for i in range(2)]
            hb = P.sb("hb", [128, 4, D], BF16)
            hTs = [P.sb(f"hT{i}", [128, 8, 512], BF16) for i in range(2)]
            ptr = [P.ps(f"ptr{i}", [128, 512], BF16) for i in range(2)]
            ppf = [P.ps(f"ppf{i}", [128, 512]) for i in range(3)]
            ppt = [P.ps(f"ppt{i}", [128, 512]) for i in range(2)]
            stg = [P.sb(f"stg{i}", [64, 16, 512], BF16) for i in range(2)]
            stgV = P.sb("stgV", [128, 12, 4, 65], BF16)
            P.memset("pool", stgV[:, :, :, 64:65], 1.0)
            t1 = [P.sb(f"rt1{i}", [16, 512], F32) for i in range(2)]
            t2 = [P.sb(f"rt2{i}", [16, 512], F32) for i in range(2)]
            ef = P.sb("ef", [4, 512], F32)
            gtmp = P.sb("gtmp", [128, 24], F32)
            nrope = 0
            for tt in range(NTT_B):
                tok = slice(tt * 512, (tt + 1) * 512)
                hT = hTs[tt % 2]
                cosT = cosTs[tt % 2]
                sinT = sinTs[tt % 2]
                P.dma("sp", cosT, self.TAB[0][0:16, tok])
                P.dma("sp", sinT, self.TAB[1][0:16, tok])
                if SUBB >= 1:
                    self.norm_transpose(tt, xsrc, gbc, xts, hb, hT, ptr, junk, small)
                for t in range(NTILES if SUBB >= 2 else 0):
                    c0, M, roped = TINFO[t]
                    pp = ppf[t % 3]
                    for kc in range(8):
                        P.mm(pp, wi[:, kc, c0:c0 + 128], hT[:, kc, :], start=(kc == 0), stop=(kc == 7))
                    st = stg[t // 16]
                    dstv = st[:, t % 16, :]
                    P.copy("dve" if (roped and SUBB >= 3) else "act", dstv, pp[0:64])
                    if roped and SUBB >= 3:
                        a1 = t1[nrope % 2]
                        a2 = t2[nrope % 2]
                        nrope += 1
                        P.tt("dve", a1, pp[0:16], cosT[0:16, :], ALU.mult)
                        P.tt("dve", a2, pp[64:80], sinT[0:16, :], ALU.mult)
                        P.tt("dve", dstv[0:16], a1, a2, ALU.add)
                    if t % 16 == 15 and SUBB >= 6:
                        h0 = (t // 16) * 16
                        P.dma("sp", self.QK[0:64, h0:h0 + 16, tok], st)
                pp = ppf[NTILES % 3]
                if SUBB >= 4:
                    for kc in range(8):
                        P.mm(pp, wi[:, kc, C_FF:C_FF + 128], hT[:, kc, :], start=(kc == 0), stop=(kc == 7))
                    P.act(ef, pp[0:4], AF.Exp, bias=negb, scale=-1.0)
                    P.act(lnf[:, tok], ef, AF.Ln, bias=self.onec[0:4], scale=1.0)
                for s in range(4 if SUBB >= 5 else 0):
                    blk = 4 * tt + s
                    pv = ppt[0]
                    for kc in range(8):
                        P.mm(pv, hT[:, kc, s * 128:(s + 1) * 128], wi[:, kc, C_TM1:C_TM1 + 512], start=(kc == 0), stop=(kc == 7))
                    P.copy("act", stgV[:, 0:8, s, 0:64], pv.re("p (h e) -> p h e", e=64))
                    pv2 = ppt[1]
                    for kc in range(8):
                        P.mm(pv2[:, 0:280], hT[:, kc, s * 128:(s + 1) * 128], wi[:, kc, C_TM2:C_TM2 + 280], start=(kc == 0), stop=(kc == 7))
                    P.copy("dve", stgV[:, 8:12, s, 0:64], pv2[:, 0:256].re("p (h e) -> p h e", e=64))
                    P.tt("dve", gtmp, pv2[:, 256:280], bg, ALU.add)
                    P.act(gates[:, blk, :], gtmp, AF.Sigmoid)
                if SUBB >= 6:
                    P.dma("sp", self.VA.re("h p b e -> p h b e")[:, :, 4 * tt:4 * tt + 4, :], stgV)

    def phase_B2(self, lnf, cs):
        P = self.P
        with P.scope():
            ones = P.sb("ones4", [4, S], BF16)
            P.memset("pool", ones, 1.0)
            cumf = P.sb("cum", [128, S], F32)
            P.memset("pool", cumf, 0.0)
            cum = cumf[0:4]
            o_, l_, c_ = ones.ap, lnf.ap, cum.ap
            P.op("dve", lambda e: e.tensor_tensor_scan(c_, o_, l_, 0.0, ALU.mult, ALU.add), [ones, lnf], [cumf])
            pcs = [P.ps(f"pcs{i}", [128, 4, 128], F32) for i in range(2)]
            for b4 in range(NB // 4):
                pc = pcs[b4 % 2]
                for bb in range(4):
                    b = 4 * b4 + bb
                    P.tr(pc[:, bb, :], cumf[:, b * 128:(b + 1) * 128], self.identf)
                P.copy("dve" if b4 % 2 == 0 else "act", cs[:, 4 * b4:4 * b4 + 4, :], pc[:, :, 0:4])
            c8 = P.sb("c8", [4, S], F32)
            P.ts("dve", c8, cum, -8.0, ALU.mult)
            cj = [P.sb(f"cj{j}", [4, S], BF16) for j in range(3)]
            P.copy("dve", cj[0], c8)
            P.tt("dve", c8, c8, cj[0], ALU.subtract)
            P.copy("dve", cj[1], c8)
            P.tt("dve", c8, c8, cj[1], ALU.subtract)
            P.copy("dve", cj[2], c8)
            for j in range(3):
                P.dma("sp", self.QK[64 + j, T_FQ:T_FQ + 4, :], cj[j])
                P.dma("sp", self.QK[64 + j, T_FK:T_FK + 4, :], ones)

    def attn_setup(self):
        P = self.P
        self.sps = [P.ps(f"sps{i}", [128, 512]) for i in range(3)]
        self.accs = [P.ps(f"acc{i}", [128, 4, 65]) for i in range(2)]
        self.pts = [P.sb(f"pts{i}", [128, 512], BF16) for i in range(3)]
        self.zer = P.sb("zer", [128, 260], BF16)
        P.memset("pool", self.zer, 0.0)
        self.masks = P.sb("masks", [128, NMASK, 512], BF16)
        P.dma("sp", self.masks, self.c_masks)
        self.rot = 0
        self.arot = 0

    def zero_acc(self, acc, n):
        flat = acc.re("p s e -> p (s e)")
        self.P.mm(flat[:, 0:n], self.zer[:, 0:128], self.zer[:, 0:n], start=True, stop=False)

    def score_step(self, kT, qT, rows, extra, bias, pv_list, last):
        P = self.P
        sp = self.sps[self.rot % 3]
        pt = self.pts[self.rot % 3]
        self.rot += 1
        n = len(extra)
        P.mm(sp[0:rows], kT, qT, start=True, stop=(n == 0))
        for i, (l, r) in enumerate(extra):
            P.mm(sp[0:rows], l, r, start=False, stop=(i == n - 1))
        P.act(pt[0:rows], sp[0:rows], AF.Exp, scale=0.125, bias=bias)
        for j, (accv, s, rhs) in enumerate(pv_list):
            P.mm(accv, pt[0:rows, s * 128:(s + 1) * 128], rhs, start=False, stop=(last and j == len(pv_list) - 1))

    def mask_pair(self, idx, rows=128):
        return (self.identb[0:rows, 0:rows], self.masks[0:rows, idx, :])

    def phase_C(self, L, gates, cs):
        P = self.P
        with P.scope():
            self.attn_setup()
            rden = [P.sb(f"rden{i}", [128, 4], F32) for i in range(2)]
            mst = [P.sb(f"mst{i}", [128, 4, 64], BF16) for i in range(2)]
            self.nmst = 0

            def simple_head(tq, tk, hv, col0, qrows, kb_lo, mask_of, bias_of):
                with P.scope():
                    qa = P.sb("qa", [128, S], BF16)
                    ka = P.sb("ka", [128, S], BF16)
                    va = P.sb("va", [128, NB, 65], BF16)
                    P.memset("pool", qa[64:128], 0.0)
                    P.memset("pool", ka[64:128], 0.0)
                    P.dma("sp", qa[0:qrows], self.QK[0:qrows, tq, :])
                    P.dma("sp", ka[0:qrows], self.QK[0:qrows, tk, :])
                    P.dma("sp", va, self.VA[hv])
                    for i in range(NQT):
                        acc = self.accs[self.arot % 2]
                        self.arot += 1
                        self.zero_acc(acc, 260)
                        kbs = list(range(kb_lo(i), 4 * i + 4))
                        for kb in kbs:
                            o = kb - 4 * i
                            midx = mask_of(o)
                            extra = [] if midx is None else [self.mask_pair(midx)]
                            pv = [(acc[:, s, :], s, va[:, kb, :]) for s in range(4)
                                  if midx is None or not MSKIP[midx][s]]
                            self.score_step(ka[:, kb * 128:(kb + 1) * 128], qa[:, i * 512:(i + 1) * 512], 128,
                                            extra, bias_of(kb), pv, kb == kbs[-1])
                        rd = rden[self.nmst % 2]
                        ms = mst[self.nmst % 2]
                        self.nmst += 1
                        P.recip(rd, acc[:, :, 64])
                        for s in range(4):
                            P.ts("dve", ms[:, s, :], acc[:, s, 0:64], rd[:, s:s + 1], ALU.mult)
                        P.dma("sp", self.MIX[i * 512:(i + 1) * 512, col0:col0 + 64].re("(s p) e -> p s e", p=128), ms)

            for h in range(4):
                simple_head(T_FQ + h, T_FK + h, h, 64 * h, 67, lambda i: 0,
                            lambda o: (M_CAUSAL + o) if o >= 0 else None,
                            lambda kb, h=h: cs[:, kb, h:h + 1])
            for h in range(4 if STAGE >= 4 else 0):
                simple_head(T_DQ + h, T_DK + h, 8 + h, 768 + 64 * h, 64, lambda i: max(0, 4 * i - 16),
                            lambda o: dil_mask_index(o), lambda kb: None)
            if STAGE >= 5:
                self.nsa(L, gates, rden)

    def nsa(self, L, gates, rden):
        P = self.P
        with P.scope():
            kcmpT = P.sb("kcmpT", [128, 2, 256], BF16)
            P.memset("pool", kcmpT, 0.0)
            vca = P.sb("vca", [128, 2, 2, 65], BF16)
            P.memset("pool", vca, 0.0)
            P.memset("pool", vca[:, :, :, 64:65], 1.0)
            self.compress(L, kcmpT, vca)
            ovt = P.sb("ovt", [128, 2, 64], BF16)
            P.dma("sp", ovt, self.c_ovt)
            emat = P.sb("emat", [128, S], BF16)
            P.dma("sp", emat, self.c_emat)
            keepw = P.sb("keepw", [128, 127], F32)
            addw = P.sb("addw", [128, 127], F32)
            P.dma("sp", keepw, self.c_keepw)
            P.dma("sp", addw, self.c_addw)
            impP = P.ps("impP", [128, 4, 64])
            ptrs = P.ps("ptrs", [128, 512], BF16)
            impG = P.sb("impG", [128, 4, 64], F32)
            onsa = P.sb("onsa", [128, 4, 4, 64], F32)
            omst = [P.sb(f"omst{i}", [128, 4, 4, 64], BF16) for i in range(2)]
            sc = [P.sb(f"sc{i}", [128, 4], F32) for i in range(2)]
            impm = P.sb("impm", [128, 64], F32)
            imp2 = P.sb("imp2", [128, 64], F32)
            m8a = P.sb("m8a", [128, 8], F32)
            m8b = P.sb("m8b", [128, 8], F32)
            selb = [P.sb(f"selb{i}", [128, 128], BF16) for i in range(2)]
            for t_ in selb:
                P.memset("pool", t_, 0.0)
            selbT = P.sb("selbT", [128, 512], BF16)
            qns = [P.sb(f"qn{i}", [128, 4, 512], BF16) for i in range(2)]
            for t_ in qns:
                P.memset("pool", t_[64:128], 0.0)
            ksT = P.sb("ksT", [128, S], BF16)
            kwT = P.sb("kwT", [128, S], BF16)
            P.memset("pool", ksT[64:128], 0.0)
            P.memset("pool", kwT[64:128], 0.0)
            vsa = P.sb("vsa", [128, NB, 65], BF16)
            vwa = P.sb("vwa", [128, NB, 65], BF16)
            nq = 0
            nsc = 0
            for g in range(NSA_G if NSUB >= 2 else 0):
                P.dma("sp", ksT[0:64], self.QK[0:64, T_KS + g, :])
                P.dma("sp", kwT[0:64], self.QK[0:64, T_KW + g, :])
                P.dma("sp", vsa, self.VA[4 + g])
                P.dma("sp", vwa, self.VA[6 + g])
                for i in range(NSA_I):
                    qn = qns[nq % 2]
                    nq += 1
                    P.dma("sp", qn[0:64], self.QK[0:64, T_NQ + 4 * g:T_NQ + 4 * g + 4, i * 512:(i + 1) * 512])

                    def epilogue(acc, r, branch, first):
                        nonlocal nsc
                        h = 4 * g + r
                        rd = rden[nsc % 2]
                        scv = sc[nsc % 2]
                        nsc += 1
                        P.ts("dve", rd, acc[:, :, 64], 1e-30, ALU.max)
                        P.recip(rd, rd)
                        P.tt("dve", scv, rd, gates[:, 4 * i:4 * i + 4, 3 * h + branch], ALU.mult)
                        for s in range(4):
                            if first:
                                P.ts("dve", onsa[:, s, r, :], acc[:, s, 0:64], scv[:, s:s + 1], ALU.mult)
                            else:
                                P.stt(onsa[:, s, r, :], acc[:, s, 0:64], scv[:, s:s + 1], onsa[:, s, r, :], ALU.mult, ALU.add)
                        return rd

                    for r in range(4):
                        acc = self.accs[self.arot % 2]
                        self.arot += 1
                        self.zero_acc(acc, 260)
                        self.zero_acc(impP, 256)
                        nbs = [0] if i < 4 else [0, 1]
                        for nb in nbs:
                            rows = 128
                            v = i if nb == 0 else i - 4
                            extra = [self.mask_pair(M_CMP + v, rows)] if v <= 4 else []
                            pv = []
                            for s in range(4):
                                pv.append((acc[:, s, :], s, vca[0:rows, g, nb, :]))
                                pv.append((impP[:, s, :], s, ovt[0:rows, nb, :]))
                            self.score_step(kcmpT[:, g, nb * 128:nb * 128 + rows], qn[:, r, :], rows, extra, None, pv,
                                            nb == nbs[-1])
                        rd = epilogue(acc, r, 0, True)
                        for s in range(4):
                            if r == 0:
                                P.ts("dve", impG[:, s, :], impP[:, s, :], rd[:, s:s + 1], ALU.mult)
                            else:
                                P.stt(impG[:, s, :], impP[:, s, :], rd[:, s:s + 1], impG[:, s, :], ALU.mult, ALU.add)
                    for s in range(4 if NSUB >= 3 else 0):
                        blk = 4 * i + s
                        c0 = 63 - 2 * blk
                        P.tt("dve", impm, impG[:, s, :], keepw[:, c0:c0 + 64], ALU.mult)
                        P.tt("dve", impm, impm, addw[:, c0:c0 + 64], ALU.add)
                        P.memset("dve", impm[:, 0:1], 1e9)
                        a_, b_, c_, d_ = impm.ap, imp2.ap, m8a.ap, m8b.ap
                        P.op("dve", lambda e, a_=a_, c_=c_: e.max(c_, a_), [impm], [m8a])
                        P.op("dve", lambda e, a_=a_, b_=b_, c_=c_: e.match_replace(b_, c_, a_, -3.0e38), [impm, m8a], [imp2])
                        P.op("dve", lambda e, b_=b_, d_=d_: e.max(d_, b_), [imp2], [m8b])
                        sb_ = selb[s % 2]
                        P.ts("dve", sb_[:, 0:64], impm, m8b[:, 7:8], ALU.is_lt, NEG, ALU.mult)
                        P.tr(ptrs[:, s * 128:(s + 1) * 128], sb_, self.identb)
                    if NSUB >= 3:
                        P.copy("act", selbT, ptrs)
                    for r in range(4 if NSUB >= 4 else 0):
                        acc = self.accs[self.arot % 2]
                        self.arot += 1
                        self.zero_acc(acc, 260)
                        kbs = list(range(0, 4 * i + 4))
                        for kb in kbs:
                            o = kb - 4 * i
                            extra = [(emat[:, kb * 128:(kb + 1) * 128], selbT)]
                            midx = None
                            if o >= 0:
                                midx = M_CAUSAL + o
                                extra.append(self.mask_pair(midx))
                            pv = [(acc[:, s, :], s, vsa[:, kb, :]) for s in range(4)
                                  if midx is None or not MSKIP[midx][s]]
                            self.score_step(ksT[:, kb * 128:(kb + 1) * 128], qn[:, r, :], 128, extra, None, pv,
                                            kb == kbs[-1])
                        epilogue(acc, r, 1, False)
                    for r in range(4 if NSUB >= 5 else 0):
                        acc = self.accs[self.arot % 2]
                        self.arot += 1
                        self.zero_acc(acc, 260)
                        kbs = list(range(max(0, 4 * i - 4), 4 * i + 4))
                        for kb in kbs:
                            o = kb - 4 * i
                            midx = (M_CAUSAL + o) if o >= 0 else (M_WIN + o + 4)
                            pv = [(acc[:, s, :], s, vwa[:, kb, :]) for s in range(4) if not MSKIP[midx][s]]
                            self.score_step(kwT[:, kb * 128:(kb + 1) * 128], qn[:, r, :], 128, [self.mask_pair(midx)],
                                            None, pv, kb == kbs[-1])
                        epilogue(acc, r, 2, False)
                    om = omst[i % 2]
                    P.copy("pool", om, onsa)
                    c0 = 256 + 256 * g
                    P.dma("sp", self.MIX[i * 512:(i + 1) * 512, c0:c0 + 256].re("(s p) (r e) -> p s r e", p=128, e=64), om)

    def compress(self, L, kcmpT, vca):
        P = self.P
        with P.scope():
            srcT = [P.sb(f"cT{i}", [128, 2, S], BF16) for i in range(2)]
            w1 = [P.sb(f"w1{i}", [128, 32, 256], BF16) for i in range(2)]
            w2 = [P.sb(f"w2{i}", [128, 2, 128], BF16) for i in range(2)]
            posT = [P.sb(f"posT{i}", [128, 32], BF16) for i in range(2)]
            for i_ in range(2):
                P.memset("pool", srcT[i_][64:128], 0.0)
                P.memset("pool", w1[i_][64:128], 0.0)
                P.memset("pool", w2[i_], 0.0)
                P.memset("pool", posT[i_][64:128], 0.0)
            P.dma("sp", srcT[0][0:64], self.QK[0:64, T_KC:T_KC + 2, :])
            P.dma("sp", srcT[1][0:64], self.QK[0:64, T_VC:T_VC + 2, :])
            ab = [P.sb(f"ab{i}", [128, 2], F32) for i in range(2)]
            gl = [P.sb(f"gl{i}", [128, 2, 2, 256], BF16) for i in range(2)]
            for t_ in gl:
                P.memset("pool", t_, 0.0)
            ph = [self.sps[0], self.sps[1]]
            pa = self.sps[2][:, 0:2]
            po = [a.re("p s e -> p (s e)") for a in self.accs]
            u = [P.sb(f"cu{i}", [128, 256], F32) for i in range(2)]
            u2 = [P.sb(f"cv{i}", [128, 256], F32) for i in range(2)]
            for kv in range(2):
                for l0 in range(0, 32, 8):
                    P.dma("pool", w1[kv][0:64, l0:l0 + 8, :], self.w1[L, kv][:, l0:l0 + 8, :], max_dma_last_dim=4096)
                P.dma("pool", w2[kv][:, :, 0:64], self.w2[L, kv].re("(c p) e -> p c e", p=128))
                P.dma("pool", posT[kv][0:64], self.posT[L, kv])
            n = 0
            for kv in range(2):
                for ch in range(2):
                    for l in range(32):
                        P.mm(pa[:, ch:ch + 1], w1[kv][:, l, ch * 128:(ch + 1) * 128], posT[kv][:, l:l + 1],
                             start=(l == 0), stop=(l == 31))
                P.copy("dve", ab[kv], pa)
                for g in range(2):
                    for ch in range(2):
                        p_ = ph[n % 2]
                        uu = u[n % 2]
                        vv = u2[n % 2]
                        n += 1
                        for l in range(32):
                            P.mm(p_[:, 0:255], w1[kv][:, l, ch * 128:(ch + 1) * 128],
                                 srcT[kv][:, g, l:l + 16 * 254 + 1:16], start=(l == 0), stop=(l == 31))
                        P.act(uu[:, 0:255], p_[:, 0:255], AF.Identity, bias=ab[kv][:, ch:ch + 1], scale=1.0)
                        P.tt("dve", vv[:, 0:255], uu[:, 0:255], uu[:, 0:255], ALU.mult)
                        P.ts("dve", vv[:, 0:255], vv[:, 0:255], 0.044715, ALU.mult, 1.0, ALU.add)
                        P.tt("dve", vv[:, 0:255], vv[:, 0:255], uu[:, 0:255], ALU.mult)
                        P.act(vv[:, 0:255], vv[:, 0:255], AF.Sigmoid, scale=2.0 * math.sqrt(2.0 / math.pi))
                        P.tt("dve", gl[kv][:, g, ch, 0:255], vv[:, 0:255], uu[:, 0:255], ALU.mult)
                    if kv == 0:
                        pk = po[g % 2]
                        for ch in range(2):
                            P.mm(pk[:, 0:255], w2[0][:, ch, :], gl[0][:, g, ch, 0:255], start=(ch == 0), stop=(ch == 1))
                        P.copy("act", kcmpT[0:64, g, 0:255], pk[0:64, 0:255])
                    else:
                        for nb in range(2):
                            rows = 128
                            pk = po[nb]
                            for ch in range(2):
                                P.mm(pk[0:rows, 0:64], gl[1][:, g, ch, nb * 128:nb * 128 + rows], w2[1][:, ch, 0:64],
                                     start=(ch == 0), stop=(ch == 1))
                            P.copy("act", vca[0:rows, g, nb, 0:64], pk[0:rows, 0:64])

    def phase_D(self, L, xsrc, xmid):
        P = self.P
        with P.scope():
            wo = P.sb("wo", [128, 8, D], BF16)
            wsrc = self.w_out[L].re("(kc p) n -> p kc n", p=128)
            for c0 in range(0, D, 512):
                P.dma("pool", wo[:, :, c0:c0 + 512], wsrc[:, :, c0:c0 + 512])
            gbc = self.gain_bc("gbc", L, 1)
            mixb = [P.sb(f"mixb{i}", [128, D], BF16) for i in range(2)]
            mT = [P.sb(f"mT{i}", [128, 8, 128], BF16) for i in range(2)]
            ptm = [P.ps(f"ptm{i}", [128, 512], BF16) for i in range(2)]
            pys = [[P.ps(f"py{i}{j}", [128, 512]) for j in range(2)] for i in range(2)]
            xts = [P.sb(f"xt{i}", [128, D], F32) for i in range(2)]
            xns = [P.sb(f"xn{i}", [128, D], F32) for i in range(2)]
            junk = P.sb("junk", [128, D], BF16)
            small = [tuple(P.sb(f"sm{i}{j}", [128, 1], F32) for j in range(4)) for i in range(2)]
            for blk in range(NB):
                mb = mixb[blk % 2]
                P.dma("sp", mb, self.MIX[blk * 128:(blk + 1) * 128, :])
                mt = mT[blk % 2]
                for hf in range(2):
                    pt = ptm[hf]
                    for k4 in range(4):
                        kc = hf * 4 + k4
                        P.tr(pt[:, k4 * 128:(k4 + 1) * 128], mb[:, kc * 128:(kc + 1) * 128], self.identb)
                    P.copy("act" if hf == 0 else "dve", mt[:, hf * 4:hf * 4 + 4, :], pt.re("p (k t) -> p k t", t=128))
                py = pys[blk % 2]
                for hf in range(2):
                    for kc in range(8):
                        P.mm(py[hf], mt[:, kc, :], wo[:, kc, hf * 512:(hf + 1) * 512], start=(kc == 0), stop=(kc == 7))
                self.post_norm_residual(blk, py, gbc, xsrc, xmid, xts, xns, small, junk)

    def phase_E1(self, L, xmid):
        P = self.P
        with P.scope():
            wu = P.sb("wu", [128, 8, 2 * DFF], BF16)
            wsrc = self.w_up[L].re("(kc p) n -> p kc n", p=128)
            for c0 in range(0, 2 * DFF, 704):
                P.dma("pool", wu[:, :, c0:c0 + 704], wsrc[:, :, c0:c0 + 704])
            gbc = self.gain_bc("gbc", L, 2)
            cw = P.sb("cw", [128, 44, 3], F32)
            cb = P.sb("cb", [128, 44], F32)
            P.dma("sp", cw, self.cw[L])
            P.dma("sp", cb, self.cb[L])
            halo = P.sb("halo", [128, 44, 2], F32)
            P.memset("pool", halo, 0.0)
            xts = [P.sb(f"xt{i}", [128, D], F32) for i in range(2)]
            junk = P.sb("junk", [128, D], BF16)
            small = [tuple(P.sb(f"sm{i}{j}", [128, 1], F32) for j in range(3)) for i in range(2)]
            hb = P.sb("hb", [128, 4, D], BF16)
            hTs = [P.sb(f"hT{i}", [128, 8, 512], BF16) for i in range(2)]
            ptr = [P.ps(f"ptr{i}", [128, 512], BF16) for i in range(2)]
            pus = [P.ps(f"pu{i}", [128, 512]) for i in range(4)]
            ub = [P.sb(f"ub{i}", [128, 514], F32) for i in range(4)]
            cbuf = [P.sb(f"cbuf{i}", [128, 512], F32) for i in range(4)]
            sa = [P.sb(f"sa{i}", [128, 512], F32) for i in range(2)]
            gst = [P.sb(f"gst{i}", [128, 11, 512], BF16) for i in range(2)]
            n = 0
            for tt in range(NQT):
                tok = slice(tt * 512, (tt + 1) * 512)
                hT = hTs[tt % 2]
                self.norm_transpose(tt, xmid, gbc, xts, hb, hT, ptr, junk, small)
                for j in range(22):
                    gs = gst[j // 11]
                    cc = []
                    for which in range(2):
                        ch = j + 22 * which
                        pu = pus[n % 4]
                        u = ub[n % 4]
                        c = cbuf[n % 4]
                        n += 1
                        for kc in range(8):
                            P.mm(pu, wu[:, kc, ch * 128:(ch + 1) * 128], hT[:, kc, :], start=(kc == 0), stop=(kc == 7))
                        P.copy("pool", u[:, 0:2], halo[:, ch, :])
                        P.copy("dve", u[:, 2:514], pu)
                        P.copy("pool", halo[:, ch, :], u[:, 512:514])
                        P.act(c, u[:, 2:514], AF.Identity, bias=cb[:, ch:ch + 1], scale=cw[:, ch, 2:3])
                        P.stt(c, u[:, 1:513], cw[:, ch, 1:2], c, ALU.mult, ALU.add)
                        P.stt(c, u[:, 0:512], cw[:, ch, 0:1], c, ALU.mult, ALU.add)
                        cc.append(c)
                    s_ = sa[j % 2]
                    P.act(s_, cc[0], AF.Silu)
                    P.tt("pool", gs[:, j % 11, :], s_, cc[1], ALU.mult)
                    if j % 11 == 10:
                        j0 = j - 10
                        P.dma("sp", self.GT.re("j p t -> p j t")[:, j0:j0 + 11, tok], gs)

    def phase_E2(self, L, xmid, xdst):
        P = self.P
        with P.scope():
            wd = P.sb("wd", [128, 22, D], BF16)
            wsrc = self.w_down[L].re("(j p) n -> p j n", p=128)
            for c0 in range(0, D, 512):
                P.dma("pool", wd[:, :, c0:c0 + 512], wsrc[:, :, c0:c0 + 512])
            gbc = self.gain_bc("gbc", L, 3)
            gts = [P.sb(f"gt{i}", [128, 22, 512], BF16) for i in range(2)]
            pys = [[P.ps(f"py{i}{j}", [128, 512]) for j in range(2)] for i in range(2)]
            xts = [P.sb(f"xt{i}", [128, D], F32) for i in range(2)]
            xns = [P.sb(f"xn{i}", [128, D], F32) for i in range(2)]
            junk = P.sb("junk", [128, D], BF16)
            small = [tuple(P.sb(f"sm{i}{j}", [128, 1], F32) for j in range(4)) for i in range(2)]
            for tt in range(NQT):
                gt = gts[tt % 2]
                P.dma("sp", gt, self.GT.re("j p t -> p j t")[:, :, tt * 512:(tt + 1) * 512])
                for s in range(4):
                    blk = 4 * tt + s
                    py = pys[blk % 2]
                    for hf in range(2):
                        for j in range(22):
                            P.mm(py[hf], gt[:, j, s * 128:(s + 1) * 128], wd[:, j, hf * 512:(hf + 1) * 512],
                                 start=(j == 0), stop=(j == 21))
                    self.post_norm_residual(blk, py, gbc, xmid, xdst, xts, xns, small, junk)


_NC_CACHE = {}


def _get_nc(nlayers, dbg=False):
    key = (nlayers, dbg)
    if key not in _NC_CACHE:
        _NC_CACHE[key] = Net(nlayers, dbg).build()
    return _NC_CACHE[key]


def _layer_inputs(w, ls):
    f = lambda a: np.ascontiguousarray(np.asarray(a, np.float32))
    d = {}
    d["w_in"] = f(w["w_in"][ls][:, :, COLS])
    d["w_out"] = f(w["w_out"][ls])
    d["w_up"] = f(w["w_up"][ls])
    d["w_down"] = f(w["w_down"][ls])
    d["norms"] = f(np.stack([w["attn_pre_norm"][ls], w["attn_post_norm"][ls], w["ffn_pre_norm"][ls],
                             w["ffn_post_norm"][ls]], axis=1))
    d["b_forget"] = f(w["b_forget"][ls][:, :, None])
    d["b_gate"] = f(w["b_nsa_gate"][ls][:, None, :])
    d["posT"] = f(np.stack([np.transpose(w["cmp_pos_k"][ls], (0, 2, 1)), np.transpose(w["cmp_pos_v"][ls], (0, 2, 1))], axis=1))
    w1 = np.stack([w["cmp_w1_k"][ls], w["cmp_w1_v"][ls]], axis=1)
    n = w1.shape[0]
    d["w1"] = f(w1.reshape(n, 2, 32, 64, 256).transpose(0, 1, 3, 2, 4))
    d["w2"] = f(np.stack([w["cmp_w2_k"][ls], w["cmp_w2_v"][ls]], axis=1))
    cw = np.asarray(w["conv_w"][ls], np.float32)
    d["cw"] = f(cw.reshape(n, 3, 44, 128).transpose(0, 3, 2, 1))
    d["cb"] = f(np.asarray(w["conv_b"][ls], np.float32).reshape(n, 44, 128).transpose(0, 2, 1))
    return d


def run_layers(x, positions, w, ls, dbg=False, ncores=8):
    nc = _get_nc(len(ls), dbg)
    wl = _layer_inputs(w, ls)
    in_maps = []
    for c in range(ncores):
        m = {"x": np.ascontiguousarray(x[c], dtype=np.float32),
             "pos": np.ascontiguousarray(positions[c].reshape(1, S).astype(np.int32))}
        m.update(wl)
        m.update(CONSTS)
        if dbg and STAGE < 6:
            for kk in ("w_out", "w_up", "w_down"):
                m.pop(kk)
        in_maps.append(m)
    res = run_bass_kernel_spmd(nc, in_maps, core_ids=list(range(ncores)))
    return res


FUSED = False


def kernel(**inputs):
    x = np.asarray(inputs["x"], np.float32)
    positions = np.asarray(inputs["positions"])
    w = {k: np.asarray(v) for k, v in inputs.items() if k not in ("x", "positions")}
    if FUSED:
        res = run_layers(x, positions, w, list(range(DEPTH)))
        return np.stack([r["y"] for r in res.results], axis=0).astype(np.float32)
    cur = x
    for L in range(DEPTH):
        res = run_layers(cur, positions, w, [L])
        cur = np.stack([r["y"] for r in res.results], axis=0).astype(np.float32)
    return cur
```

```python
import math
from contextlib import ExitStack, contextmanager

import numpy as np
import ml_dtypes
import concourse.bass as bass
import concourse.mybir as mybir
from concourse.bass_utils import run_bass_kernel_spmd

F32 = mybir.dt.float32
BF16 = mybir.dt.bfloat16
I32 = mybir.dt.int32
AF = mybir.ActivationFunctionType
ALU = mybir.AluOpType

S = 4096
D = 1024
NB = 32
NQT = 8
DFF = 2816
DEPTH = 4
STAGE = 8
UID0 = 0
NTT_B = 8
SUBB = 9
NSUB = 9
NSA_I = 8
NSA_G = 2
NEG = -30000.0
IN_SPLITS = (256,) * 3 + (4,) + (512,) + (128,) * 6 + (24,) + (256,) * 3
T_FQ, T_FK, T_NQ, T_KC, T_KS, T_KW, T_DQ, T_DK, T_VC = 0, 4, 8, 16, 18, 20, 22, 26, 30
NTILES = 32
M_CAUSAL, M_WIN, M_DIL, M_CMP, NMASK = 0, 4, 8, 21, 26


def dil_mask_index(o):
    if o <= -13:
        return M_DIL + (o + 16)
    if o <= -5:
        return M_DIL + 4
    return M_DIL + 5 + (o + 4)


def _col_layout():
    off = np.cumsum((0,) + IN_SPLITS)
    fq, fk, fv, ff, nq, kc, vc, ks, vs, kw, vw, ng, dq, dk, dv = [int(v) for v in off[:15]]
    tiles = []
    tiles += [(fq + 64 * h, False) for h in range(4)]
    tiles += [(fk + 64 * h, False) for h in range(4)]
    tiles += [(nq + 64 * h, True) for h in range(8)]
    tiles += [(kc + 64 * g, True) for g in range(2)]
    tiles += [(ks + 64 * g, True) for g in range(2)]
    tiles += [(kw + 64 * g, True) for g in range(2)]
    tiles += [(dq + 64 * h, True) for h in range(4)]
    tiles += [(dk + 64 * h, True) for h in range(4)]
    tiles += [(vc + 64 * g, False) for g in range(2)]
    cols = []
    tinfo = []
    for c0, roped in tiles:
        start = len(cols)
        cols += list(range(c0, c0 + 64))
        if roped:
            cols += list(range(c0 + 8, c0 + 16)) + list(range(c0, c0 + 8))
        tinfo.append((start, 80 if roped else 64, roped))
    c_ff = len(cols)
    cols += list(range(ff, ff + 4))
    c_tm1 = len(cols)
    cols += list(range(fv, fv + 256)) + list(range(vs, vs + 128)) + list(range(vw, vw + 128))
    c_tm2 = len(cols)
    cols += list(range(dv, dv + 256)) + list(range(ng, ng + 24))
    return np.asarray(cols, np.int64), tinfo, c_ff, c_tm1, c_tm2


COLS, TINFO, C_FF, C_TM1, C_TM2 = _col_layout()
NC_EXT = len(COLS)


def _masks():
    k = np.arange(128)[:, None].astype(np.int64)
    q = np.arange(512)[None, :].astype(np.int64)
    m = np.zeros((NMASK, 128, 512), np.float32)
    for o in range(4):
        m[M_CAUSAL + o] = np.where(q - 128 * o - k >= 0, 0.0, NEG)
        m[M_WIN + o] = np.where(k + 128 * o - q > 0, 0.0, NEG)
    seen = {}
    for o in range(-16, 4):
        dl = q - k - 128 * o
        mult = ((dl >= 0) & (dl <= 128)).astype(np.int64) + ((dl >= 0) & (dl <= 512) & (dl % 4 == 0)) \
            + ((dl >= 0) & (dl <= 2048) & (dl % 16 == 0))
        val = np.where(mult > 0, 8.0 * np.log(np.maximum(mult, 1)), NEG).astype(np.float32)
        idx = dil_mask_index(o)
        if idx in seen:
            assert np.array_equal(seen[idx], val), o
        seen[idx] = val
        m[idx] = val
    for v in range(5):
        m[M_CMP + v] = np.where(16 * k + 31 - 512 * v <= q, 0.0, NEG)
    return m


MASKS = _masks()
MSKIP = (MASKS.reshape(NMASK, 128, 4, 128).max(axis=(1, 3)) <= NEG + 1)


def _consts():
    c = {}
    c["masks"] = np.ascontiguousarray(MASKS.transpose(1, 0, 2)).astype(ml_dtypes.bfloat16)
    E = np.zeros((128, S), np.float32)
    E[np.arange(S) // 64, np.arange(S)] = 1.0
    c["emat"] = E.astype(ml_dtypes.bfloat16)
    c["identb"] = np.eye(128, dtype=np.float32).astype(ml_dtypes.bfloat16)
    c["identf"] = np.eye(128, dtype=np.float32)
    n = (np.arange(256).reshape(2, 128).T)[:, :, None]
    j = np.arange(64)[None, None, :]
    ov = ((16 * n < 64 * j + 64) & (16 * n + 32 > 64 * j) & (n < 255)).astype(np.float32)
    c["ovt"] = ov.astype(ml_dtypes.bfloat16)
    p = np.arange(128)[:, None]
    jj = np.arange(127)[None, :] - 63
    cur = (p >= 64).astype(np.int64)
    forced = (jj == cur) | (jj == cur - 1)
    future = jj > cur
    keep = np.where(forced | future, 0.0, 1.0).astype(np.float32)
    add = np.where(future, -1e30, np.where(forced, 1e9, 0.0)).astype(np.float32)
    c["keepw"] = keep
    c["addw"] = add
    half = 8
    inv = (500000.0 ** (-2.0 * np.arange(half, dtype=np.float32) / 16.0)).astype(np.float32)
    rc = np.zeros((80, 2), np.float32)
    for base in (0, 64):
        for i in range(16):
            rc[base + i, 0] = inv[i % 8]
            rc[base + i, 1] = -1.0 if i < 8 else 1.0
    c["ropec"] = rc
    return c


CONSTS = _consts()


class Buf:
    __slots__ = ("name", "w", "r", "psum")

    def __init__(self, name, psum=False):
        self.name = name
        self.w = {}
        self.r = {}
        self.psum = psum


class V:
    __slots__ = ("ap", "buf")

    def __init__(self, ap, buf):
        self.ap = ap
        self.buf = buf

    def __getitem__(self, idx):
        return V(self.ap[idx], self.buf)

    def re(self, pat, **kw):
        return V(self.ap.rearrange(pat, **kw), self.buf)

    def bc(self, shape):
        return V(self.ap.to_broadcast(list(shape)), self.buf)


class Prog:
    ENG = ("pe", "act", "dve", "pool", "sp")
    NDMA = 8

    def __init__(self, nc, es):
        self.nc = nc
        self.es = es
        self.ops = {e: [] for e in self.ENG}
        self.cnt = {e: 0 for e in self.ENG}
        self.keysem = {e: es.enter_context(nc.semaphore("s_" + e)) for e in self.ENG}
        self.dsem = {}
        for q in ("sp", "pool"):
            self.dsem[q] = []
            for i in range(self.NDMA):
                sem = es.enter_context(nc.semaphore(f"d_{q}{i}"))
                self.keysem[("d", q, i)] = sem
                self.dsem[q].append([sem, 0])
        self.drr = {q: 0 for q in self.dsem}
        self.waited = {e: {} for e in self.ENG}
        self.pending = {e: {} for e in self.ENG}
        self.uid = UID0

    def _name(self, name):
        self.uid += 1
        return f"{name}_{self.uid}"

    def sb(self, name, shape, dt):
        t = self.es.enter_context(self.nc.sbuf_tensor(self._name(name), list(shape), dt))
        return V(t[:], Buf(name))

    def ps(self, name, shape, dt=F32):
        full = 512 if dt == F32 else 1024
        t = self.es.enter_context(self.nc.psum_tensor(self._name(name), [128, full], dt))
        shape = list(shape)
        n = int(np.prod(shape[1:]))
        assert n <= full
        ap = t[:][0:shape[0], 0:n]
        if len(shape) == 3:
            ap = ap.rearrange("p (a b) -> p a b", b=shape[2])
        elif len(shape) == 4:
            ap = ap.rearrange("p (a b c) -> p a b c", b=shape[2], c=shape[3])
        return V(ap, Buf(name, psum=True))

    def dram(self, name, shape, dt, kind="Internal"):
        t = self.nc.dram_tensor(name, list(shape), dt, kind=kind)
        return V(t.ap(), Buf(name))

    def barrier(self):
        snap = {e: self.cnt[e] for e in self.ENG if self.cnt[e] > 0}
        for q, slots in self.dsem.items():
            for i, (sem, val) in enumerate(slots):
                if val > 0:
                    snap[("d", q, i)] = val
        for e in self.ENG:
            p = self.pending[e]
            for k, v in snap.items():
                if p.get(k, 0) < v:
                    p[k] = v

    @contextmanager
    def scope(self):
        self.barrier()
        old = self.es
        with ExitStack() as es:
            self.es = es
            yield
            self.barrier()
        self.es = old

    def _deps(self, eng, reads, writes):
        deps = self.pending[eng]
        self.pending[eng] = {}

        def mer(d):
            for k, v in d.items():
                if deps.get(k, 0) < v:
                    deps[k] = v
        for v in reads:
            mer(v.buf.w)
            if v.buf.psum:
                mer({k: c for k, c in v.buf.r.items() if k != eng})
        for v in writes:
            mer(v.buf.w)
            mer(v.buf.r)
        w = self.waited[eng]
        out = []
        for k, v in deps.items():
            if k == "pe" and eng == "pe":
                continue
            if w.get(k, 0) < v:
                w[k] = v
                out.append((k, v))
        return out

    def _mark(self, key, val, reads, writes):
        for v in reads:
            if v.buf.r.get(key, 0) < val:
                v.buf.r[key] = val
        for v in writes:
            if v.buf.w.get(key, 0) < val:
                v.buf.w[key] = val

    def op(self, eng, fn, reads, writes):
        waits = self._deps(eng, reads, writes)
        self.cnt[eng] += 1
        c = self.cnt[eng]
        self.ops[eng].append((waits, fn, eng))
        self._mark(eng, c, reads, writes)

    def dma(self, q, out, in_, **kw):
        i = self.drr[q]
        slot = self.dsem[q][i]
        key = ("d", q, i)
        self.drr[q] = (i + 1) % self.NDMA
        waits = self._deps(q, [in_], [out])
        if slot[1] > 0 and self.waited[q].get(key, 0) < slot[1]:
            self.waited[q][key] = slot[1]
            waits.append((key, slot[1]))
        slot[1] += 16
        oap, iap = out.ap, in_.ap
        self.ops[q].append((waits, lambda e: e.dma_start(out=oap, in_=iap, **kw), key))
        self._mark(key, slot[1], [in_], [out])

    def wait_all(self, eng, vs):
        waits = self._deps(eng, vs, [])
        self.ops[eng].append((waits, None, None))

    def emit(self):
        nc = self.nc
        with nc.Block() as block:
            def run(e, name):
                for waits, fn, inc in self.ops[name]:
                    for k, v in waits:
                        e.wait_ge(self.keysem[k], v)
                    if fn is None:
                        continue
                    ins = fn(e)
                    ins.then_inc(self.keysem[inc], 16 if isinstance(inc, tuple) else 1)

            @block.tensor
            def _(e):
                run(e, "pe")

            @block.scalar
            def _(e):
                run(e, "act")

            @block.vector
            def _(e):
                run(e, "dve")

            @block.gpsimd
            def _(e):
                run(e, "pool")

            @block.sync
            def _(e):
                run(e, "sp")

    def mm(self, out, lhsT, rhs, start=True, stop=True):
        o, l, r = out.ap, lhsT.ap, rhs.ap
        self.op("pe", lambda e: e.matmul(o, l, r, start=start, stop=stop), [lhsT, rhs], [out])

    def tr(self, out, in_, ident):
        o, i, d = out.ap, in_.ap, ident.ap
        self.op("pe", lambda e: e.transpose(o, i, d), [in_, ident], [out])

    def act(self, out, in_, func, bias=None, scale=None, accum_out=None):
        kw = {}
        rd = [in_]
        wr = [out]
        if bias is not None:
            if isinstance(bias, V):
                kw["bias"] = bias.ap
                rd.append(bias)
            else:
                kw["bias"] = bias
        if scale is not None:
            if isinstance(scale, V):
                kw["scale"] = scale.ap
                rd.append(scale)
            else:
                kw["scale"] = scale
        if accum_out is not None:
            kw["accum_out"] = accum_out.ap
            wr.append(accum_out)
        o, i = out.ap, in_.ap
        self.op("act", lambda e: e.activation(o, i, func, **kw), rd, wr)

    def ts(self, eng, out, in0, s1, op0, s2=None, op1=None):
        rd = [in0]
        a1 = s1.ap if isinstance(s1, V) else s1
        a2 = s2.ap if isinstance(s2, V) else s2
        if isinstance(s1, V):
            rd.append(s1)
        if isinstance(s2, V):
            rd.append(s2)
        kw = {}
        if op1 is not None:
            kw["op1"] = op1
        o, i = out.ap, in0.ap
        self.op(eng, lambda e: e.tensor_scalar(o, i, a1, a2, op0, **kw), rd, [out])

    def tt(self, eng, out, in0, in1, op):
        o, a, b = out.ap, in0.ap, in1.ap
        self.op(eng, lambda e: e.tensor_tensor(o, a, b, op), [in0, in1], [out])

    def stt(self, out, in0, scalar, in1, op0, op1):
        rd = [in0, in1]
        s = scalar.ap if isinstance(scalar, V) else scalar
        if isinstance(scalar, V):
            rd.append(scalar)
        o, a, b = out.ap, in0.ap, in1.ap
        self.op("dve", lambda e: e.scalar_tensor_tensor(o, a, s, b, op0, op1), rd, [out])

    def copy(self, eng, out, in_):
        o, i = out.ap, in_.ap
        if eng == "act":
            self.op("act", lambda e: e.copy(o, i), [in_], [out])
        else:
            self.op(eng, lambda e: e.tensor_copy(o, i), [in_], [out])

    def memset(self, eng, out, val):
        o = out.ap
        self.op(eng, lambda e: e.memset(o, val), [], [out])

    def recip(self, out, in_):
        o, i = out.ap, in_.ap
        self.op("dve", lambda e: e.reciprocal(o, i), [in_], [out])


class Net:
    def __init__(self, nlayers, dbg=False):
        self.nl = nlayers
        self.dbg = dbg
        self.nc = bass.Bass("TRN2", target_bir_lowering=False)
        self.es = ExitStack()

    def build(self):
        nc = self.nc
        nl = self.nl
        with self.es:
            P = self.P = Prog(nc, self.es)
            ext = lambda n, s, d: P.dram(n, s, d, kind="ExternalInput")
            self.x_in = ext("x", [S, D], F32)
            self.pos = ext("pos", [1, S], I32)
            self.w_in = ext("w_in", [nl, D, NC_EXT], F32)
            small_dbg = self.dbg and STAGE < 6
            if small_dbg:
                self.w_out = P.dram("w_out", [nl, D, D], F32)
                self.w_up = P.dram("w_up", [nl, D, 2 * DFF], F32)
                self.w_down = P.dram("w_down", [nl, DFF, D], F32)
            else:
                self.w_out = ext("w_out", [nl, D, D], F32)
                self.w_up = ext("w_up", [nl, D, 2 * DFF], F32)
                self.w_down = ext("w_down", [nl, DFF, D], F32)
            self.norms = ext("norms", [nl, 4, D], F32)
            self.b_forget = ext("b_forget", [nl, 4, 1], F32)
            self.b_gate = ext("b_gate", [nl, 1, 24], F32)
            self.posT = ext("posT", [nl, 2, 64, 32], F32)
            self.w1 = ext("w1", [nl, 2, 64, 32, 256], F32)
            self.w2 = ext("w2", [nl, 2, 256, 64], F32)
            self.cw = ext("cw", [nl, 128, 44, 3], F32)
            self.cb = ext("cb", [nl, 128, 44], F32)
            self.c_masks = ext("masks", [128, NMASK, 512], BF16)
            self.c_emat = ext("emat", [128, S], BF16)
            self.c_identb = ext("identb", [128, 128], BF16)
            self.c_identf = ext("identf", [128, 128], F32)
            self.c_ovt = ext("ovt", [128, 2, 64], BF16)
            self.c_keepw = ext("keepw", [128, 127], F32)
            self.c_addw = ext("addw", [128, 127], F32)
            self.c_ropec = ext("ropec", [80, 2], F32)
            self.x_out = P.dram("y", [S, D] if not small_dbg else [128, 8], F32, kind="ExternalOutput")
            k = "ExternalOutput" if self.dbg else "Internal"
            k2 = "ExternalOutput" if (self.dbg and not small_dbg) else "Internal"
            self.QK = P.dram("qk_scr", [68, NTILES, S], BF16, kind=k)
            self.VA = P.dram("va_scr", [12, 128, NB, 65], BF16, kind=k)
            self.MIX = P.dram("mix_scr", [S, D], BF16, kind=k if STAGE >= 3 else "Internal")
            self.GT = P.dram("gt_scr", [22, 128, S], BF16, kind="Internal")
            self.TAB = P.dram("tab_scr", [2, 80, S], F32, kind=k)
            self.XA = P.dram("xa_scr", [S, D], F32, kind=k2)
            self.XB = P.dram("xb_scr", [S, D], F32, kind="Internal")

            self.identb = P.sb("identb", [128, 128], BF16)
            self.identf = P.sb("identf", [128, 128], F32)
            P.dma("sp", self.identb, self.c_identb)
            P.dma("sp", self.identf, self.c_identf)

            self.rope_tables()
            src = self.x_in
            for L in range(nl):
                last = L == nl - 1
                mid = self.XA
                dst = self.x_out if last else self.XB
                self.layer(L, src, mid, dst)
                src = dst
            if not small_dbg:
                P.wait_all("sp", [self.x_out])
            P.emit()
        return nc

    def rope_tables(self):
        P = self.P
        with P.scope():
            rc = P.sb("ropec", [80, 2], F32)
            P.dma("sp", rc, self.c_ropec)
            pi = P.sb("posi", [80, S], I32)
            P.dma("sp", pi, self.pos.bc([80, S]))
            ang = P.sb("ang", [80, S], F32)
            P.copy("dve", ang, pi)
            P.ts("dve", ang, ang, rc[:, 0:1], ALU.mult)
            tmp = P.sb("rtmp", [80, S], F32)
            ki = P.sb("rki", [80, S], I32)
            kf = P.sb("rkf", [80, S], F32)
            res = P.sb("rres", [80, S], F32)
            TWO_PI = 2.0 * math.pi
            C1 = 6.28125
            C2 = TWO_PI - C1
            for which in range(2):
                src = ang
                if which == 0:
                    P.ts("dve", tmp, ang, math.pi / 2.0, ALU.add)
                    src = tmp
                P.ts("dve", kf, src, 1.0 / TWO_PI, ALU.mult)
                P.copy("dve", ki, kf)
                P.copy("dve", kf, ki)
                P.stt(res, kf, -C1, src, ALU.mult, ALU.add)
                P.stt(res, kf, -C2, res, ALU.mult, ALU.add)
                P.ts("dve", kf, res, math.pi, ALU.is_gt)
                P.stt(res, kf, -TWO_PI, res, ALU.mult, ALU.add)
                P.ts("dve", kf, res, -math.pi, ALU.is_lt)
                P.stt(res, kf, TWO_PI, res, ALU.mult, ALU.add)
                P.ts("dve", res, res, math.pi, ALU.min, -math.pi, ALU.max)
                P.act(res, res, AF.Sin)
                if which == 1:
                    P.ts("dve", res, res, rc[:, 1:2], ALU.mult)
                P.dma("sp", self.TAB[which], res)

    def load_cast(self, dst, src, nfree_split):
        P = self.P
        n = dst.ap.shape[1]
        step = (n + nfree_split - 1) // nfree_split
        for a in range(0, n, step):
            b = min(n, a + step)
            P.dma("pool", dst[:, a:b], src[:, a:b], max_dma_last_dim=4096)

    def rms_rstd(self, ss, rstd, sd):
        P = self.P
        P.act(sd, ss, AF.Sqrt, bias=self.epsc, scale=1.0 / D)
        P.recip(rstd, sd)

    def norm_transpose(self, tt, xsrc, gbc, xts, hb, hT, ptr, junk, small):
        P = self.P
        for s in range(4):
            blk = 4 * tt + s
            xt = xts[blk % len(xts)]
            P.dma("sp", xt, xsrc[blk * 128:(blk + 1) * 128, :])
            ss, sd, rstd = small[blk % 2]
            P.act(junk, xt, AF.Square, accum_out=ss)
            self.rms_rstd(ss, rstd, sd)
            P.stt(hb[:, s, :], xt, rstd, gbc, ALU.mult, ALU.mult)
        for kc in range(8):
            pt = ptr[kc % 2]
            for s in range(4):
                P.tr(pt[:, s * 128:(s + 1) * 128], hb[:, s, kc * 128:(kc + 1) * 128], self.identb)
            P.copy("act" if kc % 2 == 0 else "dve", hT[:, kc, :], pt)

    def post_norm_residual(self, blk, py, gbc, xsrc, xdst, xts, xns, small, junk):
        P = self.P
        ss0, ss1, sd, rstd = small[blk % 2]
        P.act(junk[:, 0:512], py[0], AF.Square, accum_out=ss0)
        P.act(junk[:, 512:1024], py[1], AF.Square, accum_out=ss1)
        P.tt("dve", ss0, ss0, ss1, ALU.add)
        self.rms_rstd(ss0, rstd, sd)
        xt = xts[blk % len(xts)]
        P.dma("sp", xt, xsrc[blk * 128:(blk + 1) * 128, :])
        xn = xns[blk % len(xns)]
        for hf in range(2):
            sl = slice(hf * 512, (hf + 1) * 512)
            P.stt(xn[:, sl], py[hf], rstd, gbc[:, sl], ALU.mult, ALU.mult)
            P.tt("pool", xn[:, sl], xn[:, sl], xt[:, sl], ALU.add)
        P.dma("sp", xdst[blk * 128:(blk + 1) * 128, :], xn)

    def layer(self, L, xsrc, xmid, xdst):
        P = self.P
        with P.scope():
            self.epsc = P.sb("epsc", [128, 1], F32)
            P.memset("dve", self.epsc, 1e-6)
            self.onec = P.sb("onec", [128, 1], F32)
            P.memset("dve", self.onec, 1.0)
            with P.scope():
                gates = P.sb("gates", [128, NB, 24], F32)
                cs = P.sb("cs", [128, NB, 4], F32)
                with P.scope():
                    lnf = P.sb("lnf", [4, S], F32)
                    if STAGE >= 1:
                        self.phase_B(L, xsrc, lnf, gates)
                    if STAGE >= 2:
                        self.phase_B2(lnf, cs)
                if STAGE >= 3:
                    self.phase_C(L, gates, cs)
            if STAGE >= 6:
                self.phase_D(L, xsrc, xmid)
            if STAGE >= 7:
                self.phase_E1(L, xmid)
            if STAGE >= 8:
                self.phase_E2(L, xmid, xdst)

    def gain_bc(self, name, L, which):
        P = self.P
        g = P.sb(name, [128, D], F32)
        P.dma("sp", g, self.norms[L, which:which + 1, :].bc([128, D]))
        return g

    def phase_B(self, L, xsrc, lnf, gates):
        P = self.P
        with P.scope():
            wi = P.sb("wi", [128, 8, NC_EXT], BF16)
            wsrc = self.w_in[L].re("(kc p) n -> p kc n", p=128)
            for c0 in range(0, NC_EXT, 800):
                c1 = min(NC_EXT, c0 + 800)
                P.dma("pool", wi[:, :, c0:c1], wsrc[:, :, c0:c1])
            gbc = self.gain_bc("gbc", L, 0)
            cosTs = [P.sb(f"cosT{i}", [16, 512], F32) for i in range(2)]
            sinTs = [P.sb(f"sinT{i}", [16, 512], F32) for i in range(2)]
            negb = P.sb("negb", [4, 1], F32)
            P.dma("sp", negb, self.b_forget[L])
            P.ts("dve", negb, negb, -1.0, ALU.mult)
            bg = P.sb("bg", [128, 24], F32)
            P.dma("sp", bg, self.b_gate[L].bc([128, 24]))
            xts = [P.sb(f"xt{i}", [128, D], F32) for i in range(2)]
            junk = P.sb("junk", [128, D], BF16)
            small = [tuple(P.sb(f"sm{i}{j}", [128, 1], F32) for j in range(3)) for i in range(2)]
            hb = P.sb("hb", [128, 4, D], BF16)
            hTs = [P.sb(f"hT{i}", [128, 8, 512], BF16) for i in range(2)]
            ptr = [P.ps(f"ptr{i}", [128, 512], BF16) for i in range(2)]
            ppf = [P.ps(f"ppf{i}", [128, 512]) for i in range(3)]
            ppt = [P.ps(f"ppt{i}", [128, 512]) for i in range(2)]
            stg = [P.sb(f"stg{i}", [64, 16, 512], BF16) for i in range(2)]
            stgV = P.sb("stgV", [128, 12, 4, 65], BF16)
            P.memset("pool", stgV[:, :, :, 64:65], 1.0)
            t1 = [P.sb(f"rt1{i}", [16, 512], F32) for i in range(2)]
            t2 = [P.sb(f"rt2{i}", [16, 512], F32) for i in range(2)]
            ef = P.sb("ef", [4, 512], F32)
            gtmp = P.sb("gtmp", [128, 24], F32)
            nrope = 0
            for tt in range(NTT_B):
                tok = slice(tt * 512, (tt + 1) * 512)
                hT = hTs[tt % 2]
                cosT = cosTs[tt % 2]
                sinT = sinTs[tt % 2]
                P.dma("sp", cosT, self.TAB[0][0:16, tok])
                P.dma("sp", sinT, self.TAB[1][0:16, tok])
                if SUBB >= 1:
                    self.norm_transpose(tt, xsrc, gbc, xts, hb, hT, ptr, junk, small)
                for t in range(NTILES if SUBB >= 2 else 0):
                    c0, M, roped = TINFO[t]
                    pp = ppf[t % 3]
                    for kc in range(8):
                        P.mm(pp, wi[:, kc, c0:c0 + 128], hT[:, kc, :], start=(kc == 0), stop=(kc == 7))
                    st = stg[t // 16]
                    dstv = st[:, t % 16, :]
                    P.copy("dve" if (roped and SUBB >= 3) else "act", dstv, pp[0:64])
                    if roped and SUBB >= 3:
                        a1 = t1[nrope % 2]
                        a2 = t2[nrope % 2]
                        nrope += 1
                        P.tt("dve", a1, pp[0:16], cosT[0:16, :], ALU.mult)
                        P.tt("dve", a2, pp[64:80], sinT[0:16, :], ALU.mult)
                        P.tt("dve", dstv[0:16], a1, a2, ALU.add)
                    if t % 16 == 15 and SUBB >= 6:
                        h0 = (t // 16) * 16
                        P.dma("sp", self.QK[0:64, h0:h0 + 16, tok], st)
                pp = ppf[NTILES % 3]
                if SUBB >= 4:
                    for kc in range(8):
                        P.mm(pp, wi[:, kc, C_FF:C_FF + 128], hT[:, kc, :], start=(kc == 0), stop=(kc == 7))
                    P.act(ef, pp[0:4], AF.Exp, bias=negb, scale=-1.0)
                    P.act(lnf[:, tok], ef, AF.Ln, bias=self.onec[0:4], scale=1.0)
                for s in range(4 if SUBB >= 5 else 0):
                    blk = 4 * tt + s
                    pv = ppt[0]
                    for kc in range(8):
                        P.mm(pv, hT[:, kc, s * 128:(s + 1) * 128], wi[:, kc, C_TM1:C_TM1 + 512], start=(kc == 0), stop=(kc == 7))
                    P.copy("act", stgV[:, 0:8, s, 0:64], pv.re("p (h e) -> p h e", e=64))
                    pv2 = ppt[1]
                    for kc in range(8):
                        P.mm(pv2[:, 0:280], hT[:, kc, s * 128:(s + 1) * 128], wi[:, kc, C_TM2:C_TM2 + 280], start=(kc == 0), stop=(kc == 7))
                    P.copy("dve", stgV[:, 8:12, s, 0:64], pv2[:, 0:256].re("p (h e) -> p h e", e=64))
                    P.tt("dve", gtmp, pv2[:, 256:280], bg, ALU.add)
                    P.act(gates[:, blk, :], gtmp, AF.Sigmoid)
                if SUBB >= 6:
                    P.dma("sp", self.VA.re("h p b e -> p h b e")[:, :, 4 * tt:4 * tt + 4, :], stgV)

    def phase_B2(self, lnf, cs):
        P = self.P
        with P.scope():
            ones = P.sb("ones4", [4, S], BF16)
            P.memset("pool", ones, 1.0)
            cumf = P.sb("cum", [128, S], F32)
            P.memset("pool", cumf, 0.0)
            cum = cumf[0:4]
            o_, l_, c_ = ones.ap, lnf.ap, cum.ap
            P.op("dve", lambda e: e.tensor_tensor_scan(c_, o_, l_, 0.0, ALU.mult, ALU.add), [ones, lnf], [cumf])
            pcs = [P.ps(f"pcs{i}", [128, 4, 128], F32) for i in range(2)]
            for b4 in range(NB // 4):
                pc = pcs[b4 % 2]
                for bb in range(4):
                    b = 4 * b4 + bb
                    P.tr(pc[:, bb, :], cumf[:, b * 128:(b + 1) * 128], self.identf)
                P.copy("dve" if b4 % 2 == 0 else "act", cs[:, 4 * b4:4 * b4 + 4, :], pc[:, :, 0:4])
            c8 = P.sb("c8", [4, S], F32)
            P.ts("dve", c8, cum, -8.0, ALU.mult)
            cj = [P.sb(f"cj{j}", [4, S], BF16) for j in range(3)]
            P.copy("dve", cj[0], c8)
            P.tt("dve", c8, c8, cj[0], ALU.subtract)
            P.copy("dve", cj[1], c8)
            P.tt("dve", c8, c8, cj[1], ALU.subtract)
            P.copy("dve", cj[2], c8)
            for j in range(3):
                P.dma("sp", self.QK[64 + j, T_FQ:T_FQ + 4, :], cj[j])
                P.dma("sp", self.QK[64 + j, T_FK:T_FK + 4, :], ones)

    def attn_setup(self):
        P = self.P
        self.sps = [P.ps(f"sps{i}", [128, 512]) for i in range(3)]
        self.accs = [P.ps(f"acc{i}", [128, 4, 65]) for i in range(2)]
        self.pts = [P.sb(f"pts{i}", [128, 512], BF16) for i in range(3)]
        self.zer = P.sb("zer", [128, 260], BF16)
        P.memset("pool", self.zer, 0.0)
        self.masks = P.sb("masks", [128, NMASK, 512], BF16)
        P.dma("sp", self.masks, self.c_masks)
        self.rot = 0
        self.arot = 0

    def zero_acc(self, acc, n):
        flat = acc.re("p s e -> p (s e)")
        self.P.mm(flat[:, 0:n], self.zer[:, 0:128], self.zer[:, 0:n], start=True, stop=False)

    def score_step(self, kT, qT, rows, extra, bias, pv_list, last):
        P = self.P
        sp = self.sps[self.rot % 3]
        pt = self.pts[self.rot % 3]
        self.rot += 1
        n = len(extra)
        P.mm(sp[0:rows], kT, qT, start=True, stop=(n == 0))
        for i, (l, r) in enumerate(extra):
            P.mm(sp[0:rows], l, r, start=False, stop=(i == n - 1))
        P.act(pt[0:rows], sp[0:rows], AF.Exp, scale=0.125, bias=bias)
        for j, (accv, s, rhs) in enumerate(pv_list):
            P.mm(accv, pt[0:rows, s * 128:(s + 1) * 128], rhs, start=False, stop=(last and j == len(pv_list) - 1))

    def mask_pair(self, idx, rows=128):
        return (self.identb[0:rows, 0:rows], self.masks[0:rows, idx, :])

    def phase_C(self, L, gates, cs):
        P = self.P
        with P.scope():
            self.attn_setup()
            rden = [P.sb(f"rden{i}", [128, 4], F32) for i in range(2)]
            mst = [P.sb(f"mst{i}", [128, 4, 64], BF16) for i in range(2)]
            self.nmst = 0

            def simple_head(tq, tk, hv, col0, qrows, kb_lo, mask_of, bias_of):
                with P.scope():
                    qa = P.sb("qa", [128, S], BF16)
                    ka = P.sb("ka", [128, S], BF16)
                    va = P.sb("va", [128, NB, 65], BF16)
                    P.memset("pool", qa[64:128], 0.0)
                    P.memset("pool", ka[64:128], 0.0)
                    P.dma("sp", qa[0:qrows], self.QK[0:qrows, tq, :])
                    P.dma("sp", ka[0:qrows], self.QK[0:qrows, tk, :])
                    P.dma("sp", va, self.VA[hv])
                    for i in range(NQT):
                        acc = self.accs[self.arot % 2]
                        self.arot += 1
                        self.zero_acc(acc, 260)
                        kbs = list(range(kb_lo(i), 4 * i + 4))
                        for kb in kbs:
                            o = kb - 4 * i
                            midx = mask_of(o)
                            extra = [] if midx is None else [self.mask_pair(midx)]
                            pv = [(acc[:, s, :], s, va[:, kb, :]) for s in range(4)
                                  if midx is None or not MSKIP[midx][s]]
                            self.score_step(ka[:, kb * 128:(kb + 1) * 128], qa[:, i * 512:(i + 1) * 512], 128,
                                            extra, bias_of(kb), pv, kb == kbs[-1])
                        rd = rden[self.nmst % 2]
                        ms = mst[self.nmst % 2]
                        self.nmst += 1
                        P.recip(rd, acc[:, :, 64])
                        for s in range(4):
                            P.ts("dve", ms[:, s, :], acc[:, s, 0:64], rd[:, s:s + 1], ALU.mult)
                        P.dma("sp", self.MIX[i * 512:(i + 1) * 512, col0:col0 + 64].re("(s p) e -> p s e", p=128), ms)

            for h in range(4):
                simple_head(T_FQ + h, T_FK + h, h, 64 * h, 67, lambda i: 0,
                            lambda o: (M_CAUSAL + o) if o >= 0 else None,
                            lambda kb, h=h: cs[:, kb, h:h + 1])
            for h in range(4 if STAGE >= 4 else 0):
                simple_head(T_DQ + h, T_DK + h, 8 + h, 768 + 64 * h, 64, lambda i: max(0, 4 * i - 16),
                            lambda o: dil_mask_index(o), lambda kb: None)
            if STAGE >= 5:
                self.nsa(L, gates, rden)

    def nsa(self, L, gates, rden):
        P = self.P
        with P.scope():
            kcmpT = P.sb("kcmpT", [128, 2, 256], BF16)
            P.memset("pool", kcmpT, 0.0)
            vca = P.sb("vca", [128, 2, 2, 65], BF16)
            P.memset("pool", vca, 0.0)
            P.memset("pool", vca[:, :, :, 64:65], 1.0)
            self.compress(L, kcmpT, vca)
            ovt = P.sb("ovt", [128, 2, 64], BF16)
            P.dma("sp", ovt, self.c_ovt)
            emat = P.sb("emat", [128, S], BF16)
            P.dma("sp", emat, self.c_emat)
            keepw = P.sb("keepw", [128, 127], F32)
            addw = P.sb("addw", [128, 127], F32)
            P.dma("sp", keepw, self.c_keepw)
            P.dma("sp", addw, self.c_addw)
            impP = P.ps("impP", [128, 4, 64])
            ptrs = P.ps("ptrs", [128, 512], BF16)
            impG = P.sb("impG", [128, 4, 64], F32)
            onsa = P.sb("onsa", [128, 4, 4, 64], F32)
            omst = [P.sb(f"omst{i}", [128, 4, 4, 64], BF16) for i in range(2)]
            sc = [P.sb(f"sc{i}", [128, 4], F32) for i in range(2)]
            impm = P.sb("impm", [128, 64], F32)
            imp2 = P.sb("imp2", [128, 64], F32)
            m8a = P.sb("m8a", [128, 8], F32)
            m8b = P.sb("m8b", [128, 8], F32)
            selb = [P.sb(f"selb{i}", [128, 128], BF16) for i in range(2)]
            for t_ in selb:
                P.memset("pool", t_, 0.0)
            selbT = P.sb("selbT", [128, 512], BF16)
            qns = [P.sb(f"qn{i}", [128, 4, 512], BF16) for i in range(2)]
            for t_ in qns:
                P.memset("pool", t_[64:128], 0.0)
            ksT = P.sb("ksT", [128, S], BF16)
            kwT = P.sb("kwT", [128, S], BF16)
            P.memset("pool", ksT[64:128], 0.0)
            P.memset("pool", kwT[64:128], 0.0)
            vsa = P.sb("vsa", [128, NB, 65], BF16)
            vwa = P.sb("vwa", [128, NB, 65], BF16)
            nq = 0
            nsc = 0
            for g in range(NSA_G if NSUB >= 2 else 0):
                P.dma("sp", ksT[0:64], self.QK[0:64, T_KS + g, :])
                P.dma("sp", kwT[0:64], self.QK[0:64, T_KW + g, :])
                P.dma("sp", vsa, self.VA[4 + g])
                P.dma("sp", vwa, self.VA[6 + g])
                for i in range(NSA_I):
                    qn = qns[nq % 2]
                    nq += 1
                    P.dma("sp", qn[0:64], self.QK[0:64, T_NQ + 4 * g:T_NQ + 4 * g + 4, i * 512:(i + 1) * 512])

                    def epilogue(acc, r, branch, first):
                        nonlocal nsc
                        h = 4 * g + r
                        rd = rden[nsc % 2]
                        scv = sc[nsc % 2]
                        nsc += 1
                        P.ts("dve", rd, acc[:, :, 64], 1e-30, ALU.max)
                        P.recip(rd, rd)
                        P.tt("dve", scv, rd, gates[:, 4 * i:4 * i + 4, 3 * h + branch], ALU.mult)
                        for s in range(4):
                            if first:
                                P.ts("dve", onsa[:, s, r, :], acc[:, s, 0:64], scv[:, s:s + 1], ALU.mult)
                            else:
                                P.stt(onsa[:, s, r, :], acc[:, s, 0:64], scv[:, s:s + 1], onsa[:, s, r, :], ALU.mult, ALU.add)
                        return rd

                    for r in range(4):
                        acc = self.accs[self.arot % 2]
                        self.arot += 1
                        self.zero_acc(acc, 260)
                        self.zero_acc(impP, 256)
                        nbs = [0] if i < 4 else [0, 1]
                        for nb in nbs:
                            rows = 128
                            v = i if nb == 0 else i - 4
                            extra = [self.mask_pair(M_CMP + v, rows)] if v <= 4 else []
                            pv = []
                            for s in range(4):
                                pv.append((acc[:, s, :], s, vca[0:rows, g, nb, :]))
                                pv.append((impP[:, s, :], s, ovt[0:rows, nb, :]))
                            self.score_step(kcmpT[:, g, nb * 128:nb * 128 + rows], qn[:, r, :], rows, extra, None, pv,
                                            nb == nbs[-1])
                        rd = epilogue(acc, r, 0, True)
                        for s in range(4):
                            if r == 0:
                                P.ts("dve", impG[:, s, :], impP[:, s, :], rd[:, s:s + 1], ALU.mult)
                            else:
                                P.stt(impG[:, s, :], impP[:, s, :], rd[:, s:s + 1], impG[:, s, :], ALU.mult, ALU.add)
                    for s in range(4 if NSUB >= 3 else 0):
                        blk = 4 * i + s
                        c0 = 63 - 2 * blk
                        P.tt("dve", impm, impG[:, s, :], keepw[:, c0:c0 + 64], ALU.mult)
                        P.tt("dve", impm, impm, addw[:, c0:c0 + 64], ALU.add)
                        P.memset("dve", impm[:, 0:1], 1e9)
                        a_, b_, c_, d_ = impm.ap, imp2.ap, m8a.ap, m8b.ap
                        P.op("dve", lambda e, a_=a_, c_=c_: e.max(c_, a_), [impm], [m8a])
                        P.op("dve", lambda e, a_=a_, b_=b_, c_=c_: e.match_replace(b_, c_, a_, -3.0e38), [impm, m8a], [imp2])
                        P.op("dve", lambda e, b_=b_, d_=d_: e.max(d_, b_), [imp2], [m8b])
                        sb_ = selb[s % 2]
                        P.ts("dve", sb_[:, 0:64], impm, m8b[:, 7:8], ALU.is_lt, NEG, ALU.mult)
                        P.tr(ptrs[:, s * 128:(s + 1) * 128], sb_, self.identb)
                    if NSUB >= 3:
                        P.copy("act", selbT, ptrs)
                    for r in range(4 if NSUB >= 4 else 0):
                        acc = self.accs[self.arot % 2]
                        self.arot += 1
                        self.zero_acc(acc, 260)
                        kbs = list(range(0, 4 * i + 4))
                        for kb in kbs:
                            o = kb - 4 * i
                            extra = [(emat[:, kb * 128:(kb + 1) * 128], selbT)]
                            midx = None
                            if o >= 0:
                                midx = M_CAUSAL + o
                                extra.append(self.mask_pair(midx))
                            pv = [(acc[:, s, :], s, vsa[:, kb, :]) for s in range(4)
                                  if midx is None or not MSKIP[midx][s]]
                            self.score_step(ksT[:, kb * 128:(kb + 1) * 128], qn[:, r, :], 128, extra, None, pv,
                                            kb == kbs[-1])
                        epilogue(acc, r, 1, False)
                    for r in range(4 if NSUB >= 5 else 0):
                        acc = self.accs[self.arot % 2]
                        self.arot += 1
                        self.zero_acc(acc, 260)
                        kbs = list(range(max(0, 4 * i - 4), 4 * i + 4))
                        for kb in kbs:
                            o = kb - 4 * i
                            midx = (M_CAUSAL + o) if o >= 0 else (M_WIN + o + 4)
                            pv = [(acc[:, s, :], s, vwa[:, kb, :]) for s in range(4) if not MSKIP[midx][s]]
                            self.score_step(kwT[:, kb * 128:(kb + 1) * 128], qn[:, r, :], 128, [self.mask_pair(midx)],
                                            None, pv, kb == kbs[-1])
                        epilogue(acc, r, 2, False)
                    om = omst[i % 2]
                    P.copy("pool", om, onsa)
                    c0 = 256 + 256 * g
                    P.dma("sp", self.MIX[i * 512:(i + 1) * 512, c0:c0 + 256].re("(s p) (r e) -> p s r e", p=128, e=64), om)

    def compress(self, L, kcmpT, vca):
        P = self.P
        with P.scope():
            srcT = [P.sb(f"cT{i}", [128, 2, S], BF16) for i in range(2)]
            w1 = [P.sb(f"w1{i}", [128, 32, 256], BF16) for i in range(2)]
            w2 = [P.sb(f"w2{i}", [128, 2, 128], BF16) for i in range(2)]
            posT = [P.sb(f"posT{i}", [128, 32], BF16) for i in range(2)]
            for i_ in range(2):
                P.memset("pool", srcT[i_][64:128], 0.0)
                P.memset("pool", w1[i_][64:128], 0.0)
                P.memset("pool", w2[i_], 0.0)
                P.memset("pool", posT[i_][64:128], 0.0)
            P.dma("sp", srcT[0][0:64], self.QK[0:64, T_KC:T_KC + 2, :])
            P.dma("sp", srcT[1][0:64], self.QK[0:64, T_VC:T_VC + 2, :])
            ab = [P.sb(f"ab{i}", [128, 2], F32) for i in range(2)]
            gl = [P.sb(f"gl{i}", [128, 2, 2, 256], BF16) for i in range(2)]
            for t_ in gl:
                P.memset("pool", t_, 0.0)
            ph = [self.sps[0], self.sps[1]]
            pa = self.sps[2][:, 0:2]
            po = [a.re("p s e -> p (s e)") for a in self.accs]
            u = [P.sb(f"cu{i}", [128, 256], F32) for i in range(2)]
            u2 = [P.sb(f"cv{i}", [128, 256], F32) for i in range(2)]
            for kv in range(2):
                for l0 in range(0, 32, 8):
                    P.dma("pool", w1[kv][0:64, l0:l0 + 8, :], self.w1[L, kv][:, l0:l0 + 8, :], max_dma_last_dim=4096)
                P.dma("pool", w2[kv][:, :, 0:64], self.w2[L, kv].re("(c p) e -> p c e", p=128))
                P.dma("pool", posT[kv][0:64], self.posT[L, kv])
            n = 0
            for kv in range(2):
                for ch in range(2):
                    for l in range(32):
                        P.mm(pa[:, ch:ch + 1], w1[kv][:, l, ch * 128:(ch + 1) * 128], posT[kv][:, l:l + 1],
                             start=(l == 0), stop=(l == 31))
                P.copy("dve", ab[kv], pa)
                for g in range(2):
                    for ch in range(2):
                        p_ = ph[n % 2]
                        uu = u[n % 2]
                        vv = u2[n % 2]
                        n += 1
                        for l in range(32):
                            P.mm(p_[:, 0:255], w1[kv][:, l, ch * 128:(ch + 1) * 128],
                                 srcT[kv][:, g, l:l + 16 * 254 + 1:16], start=(l == 0), stop=(l == 31))
                        P.act(uu[:, 0:255], p_[:, 0:255], AF.Identity, bias=ab[kv][:, ch:ch + 1], scale=1.0)
                        P.tt("dve", vv[:, 0:255], uu[:, 0:255], uu[:, 0:255], ALU.mult)
                        P.ts("dve", vv[:, 0:255], vv[:, 0:255], 0.044715, ALU.mult, 1.0, ALU.add)
                        P.tt("dve", vv[:, 0:255], vv[:, 0:255], uu[:, 0:255], ALU.mult)
                        P.act(vv[:, 0:255], vv[:, 0:255], AF.Sigmoid, scale=2.0 * math.sqrt(2.0 / math.pi))
                        P.tt("dve", gl[kv][:, g, ch, 0:255], vv[:, 0:255], uu[:, 0:255], ALU.mult)
                    if kv == 0:
                        pk = po[g % 2]
                        for ch in range(2):
                            P.mm(pk[:, 0:255], w2[0][:, ch, :], gl[0][:, g, ch, 0:255], start=(ch == 0), stop=(ch == 1))
                        P.copy("act", kcmpT[0:64, g, 0:255], pk[0:64, 0:255])
                    else:
                        for nb in range(2):
                            rows = 128
                            pk = po[nb]
                            for ch in range(2):
                                P.mm(pk[0:rows, 0:64], gl[1][:, g, ch, nb * 128:nb * 128 + rows], w2[1][:, ch, 0:64],
                                     start=(ch == 0), stop=(ch == 1))
                            P.copy("act", vca[0:rows, g, nb, 0:64], pk[0:rows, 0:64])

    def phase_D(self, L, xsrc, xmid):
        P = self.P
        with P.scope():
            wo = P.sb("wo", [128, 8, D], BF16)
            wsrc = self.w_out[L].re("(kc p) n -> p kc n", p=128)
            for c0 in range(0, D, 512):
                P.dma("pool", wo[:, :, c0:c0 + 512], wsrc[:, :, c0:c0 + 512])
            gbc = self.gain_bc("gbc", L, 1)
            mixb = [P.sb(f"mixb{i}", [128, D], BF16) for i in range(2)]
            mT = [P.sb(f"mT{i}", [128, 8, 128], BF16) for i in range(2)]
            ptm = [P.ps(f"ptm{i}", [128, 512], BF16) for i in range(2)]
            pys = [[P.ps(f"py{i}{j}", [128, 512]) for j in range(2)] for i in range(2)]
            xts = [P.sb(f"xt{i}", [128, D], F32) for i in range(2)]
            xns = [P.sb(f"xn{i}", [128, D], F32) for i in range(2)]
            junk = P.sb("junk", [128, D], BF16)
            small = [tuple(P.sb(f"sm{i}{j}", [128, 1], F32) for j in range(4)) for i in range(2)]
            for blk in range(NB):
                mb = mixb[blk % 2]
                P.dma("sp", mb, self.MIX[blk * 128:(blk + 1) * 128, :])
                mt = mT[blk % 2]
                for hf in range(2):
                    pt = ptm[hf]
                    for k4 in range(4):
                        kc = hf * 4 + k4
                        P.tr(pt[:, k4 * 128:(k4 + 1) * 128], mb[:, kc * 128:(kc + 1) * 128], self.identb)
                    P.copy("act" if hf == 0 else "dve", mt[:, hf * 4:hf * 4 + 4, :], pt.re("p (k t) -> p k t", t=128))
                py = pys[blk % 2]
                for hf in range(2):
                    for kc in range(8):
                        P.mm(py[hf], mt[:, kc, :], wo[:, kc, hf * 512:(hf + 1) * 512], start=(kc == 0), stop=(kc == 7))
                self.post_norm_residual(blk, py, gbc, xsrc, xmid, xts, xns, small, junk)

    def phase_E1(self, L, xmid):
        P = self.P
        with P.scope():
            wu = P.sb("wu", [128, 8, 2 * DFF], BF16)
            wsrc = self.w_up[L].re("(kc p) n -> p kc n", p=128)
            for c0 in range(0, 2 * DFF, 704):
                P.dma("pool", wu[:, :, c0:c0 + 704], wsrc[:, :, c0:c0 + 704])
            gbc = self.gain_bc("gbc", L, 2)
            cw = P.sb("cw", [128, 44, 3], F32)
            cb = P.sb("cb", [128, 44], F32)
            P.dma("sp", cw, self.cw[L])
            P.dma("sp", cb, self.cb[L])
            halo = P.sb("halo", [128, 44, 2], F32)
            P.memset("pool", halo, 0.0)
            xts = [P.sb(f"xt{i}", [128, D], F32) for i in range(2)]
            junk = P.sb("junk", [128, D], BF16)
            small = [tuple(P.sb(f"sm{i}{j}", [128, 1], F32) for j in range(3)) for i in range(2)]
            hb = P.sb("hb", [128, 4, D], BF16)
            hTs = [P.sb(f"hT{i}", [128, 8, 512], BF16) for i in range(2)]
            ptr = [P.ps(f"ptr{i}", [128, 512], BF16) for i in range(2)]
            pus = [P.ps(f"pu{i}", [128, 512]) for i in range(4)]
            ub = [P.sb(f"ub{i}", [128, 514], F32) for i in range(4)]
            cbuf = [P.sb(f"cbuf{i}", [128, 512], F32) for i in range(4)]
            sa = [P.sb(f"sa{i}", [128, 512], F32) for i in range(2)]
            gst = [P.sb(f"gst{i}", [128, 11, 512], BF16) for i in range(2)]
            n = 0
            for tt in range(NQT):
                tok = slice(tt * 512, (tt + 1) * 512)
                hT = hTs[tt % 2]
                self.norm_transpose(tt, xmid, gbc, xts, hb, hT, ptr, junk, small)
                for j in range(22):
                    gs = gst[j // 11]
                    cc = []
                    for which in range(2):
                        ch = j + 22 * which
                        pu = pus[n % 4]
                        u = ub[n % 4]
                        c = cbuf[n % 4]
                        n += 1
                        for kc in range(8):
                            P.mm(pu, wu[:, kc, ch * 128:(ch + 1) * 128], hT[:, kc, :], start=(kc == 0), stop=(kc == 7))
                        P.copy("pool", u[:, 0:2], halo[:, ch, :])
                        P.copy("dve", u[:, 2:514], pu)
                        P.copy("pool", halo[:, ch, :], u[:, 512:514])
                        P.act(c, u[:, 2:514], AF.Identity, bias=cb[:, ch:ch + 1], scale=cw[:, ch, 2:3])
                        P.stt(c, u[:, 1:513], cw[:, ch, 1:2], c, ALU.mult, ALU.add)
                        P.stt(c, u[:, 0:512], cw[:, ch, 0:1], c, ALU.mult, ALU.add)
                        cc.append(c)
                    s_ = sa[j % 2]
                    P.act(s_, cc[0], AF.Silu)
                    P.tt("pool", gs[:, j % 11, :], s_, cc[1], ALU.mult)
                    if j % 11 == 10:
                        j0 = j - 10
                        P.dma("sp", self.GT.re("j p t -> p j t")[:, j0:j0 + 11, tok], gs)

    def phase_E2(self, L, xmid, xdst):
        P = self.P
        with P.scope():
            wd = P.sb("wd", [128, 22, D], BF16)
            wsrc = self.w_down[L].re("(j p) n -> p j n", p=128)
            for c0 in range(0, D, 512):
                P.dma("pool", wd[:, :, c0:c0 + 512], wsrc[:, :, c0:c0 + 512])
            gbc = self.gain_bc("gbc", L, 3)
            gts = [P.sb(f"gt{i}", [128, 22, 512], BF16) for i in range(2)]
            pys = [[P.ps(f"py{i}{j}", [128, 512]) for j in range(2)] for i in range(2)]
            xts = [P.sb(f"xt{i}", [128, D], F32) for i in range(2)]
            xns = [P.sb(f"xn{i}", [128, D], F32) for i in range(2)]
            junk = P.sb("junk", [128, D], BF16)
            small = [tuple(P.sb(f"sm{i}{j}", [128, 1], F32) for j in range(4)) for i in range(2)]
            for tt in range(NQT):
                gt = gts[tt % 2]
                P.dma("sp", gt, self.GT.re("j p t -> p j t")[:, :, tt * 512:(tt + 1) * 512])
                for s in range(4):
                    blk = 4 * tt + s
                    py = pys[blk % 2]
                    for hf in range(2):
                        for j in range(22):
                            P.mm(py[hf], gt[:, j, s * 128:(s + 1) * 128], wd[:, j, hf * 512:(hf + 1) * 512],
                                 start=(j == 0), stop=(j == 21))
                    self.post_norm_residual(blk, py, gbc, xmid, xdst, xts, xns, small, junk)


_NC_CACHE = {}


def _get_nc(nlayers, dbg=False):
    key = (nlayers, dbg)
    if key not in _NC_CACHE:
        _NC_CACHE[key] = Net(nlayers, dbg).build()
    return _NC_CACHE[key]


def _layer_inputs(w, ls):
    f = lambda a: np.ascontiguousarray(np.asarray(a, np.float32))
    d = {}
    d["w_in"] = f(w["w_in"][ls][:, :, COLS])
    d["w_out"] = f(w["w_out"][ls])
    d["w_up"] = f(w["w_up"][ls])
    d["w_down"] = f(w["w_down"][ls])
    d["norms"] = f(np.stack([w["attn_pre_norm"][ls], w["attn_post_norm"][ls], w["ffn_pre_norm"][ls],
                             w["ffn_post_norm"][ls]], axis=1))
    d["b_forget"] = f(w["b_forget"][ls][:, :, None])
    d["b_gate"] = f(w["b_nsa_gate"][ls][:, None, :])
    d["posT"] = f(np.stack([np.transpose(w["cmp_pos_k"][ls], (0, 2, 1)), np.transpose(w["cmp_pos_v"][ls], (0, 2, 1))], axis=1))
    w1 = np.stack([w["cmp_w1_k"][ls], w["cmp_w1_v"][ls]], axis=1)
    n = w1.shape[0]
    d["w1"] = f(w1.reshape(n, 2, 32, 64, 256).transpose(0, 1, 3, 2, 4))
    d["w2"] = f(np.stack([w["cmp_w2_k"][ls], w["cmp_w2_v"][ls]], axis=1))
    cw = np.asarray(w["conv_w"][ls], np.float32)
    d["cw"] = f(cw.reshape(n, 3, 44, 128).transpose(0, 3, 2, 1))
    d["cb"] = f(np.asarray(w["conv_b"][ls], np.float32).reshape(n, 44, 128).transpose(0, 2, 1))
    return d


def run_layers(x, positions, w, ls, dbg=False, ncores=8):
    nc = _get_nc(len(ls), dbg)
    wl = _layer_inputs(w, ls)
    in_maps = []
    for c in range(ncores):
        m = {"x": np.ascontiguousarray(x[c], dtype=np.float32),
             "pos": np.ascontiguousarray(positions[c].reshape(1, S).astype(np.int32))}
        m.update(wl)
        m.update(CONSTS)
        if dbg and STAGE < 6:
            for kk in ("w_out", "w_up", "w_down"):
                m.pop(kk)
        in_maps.append(m)
    res = run_bass_kernel_spmd(nc, in_maps, core_ids=list(range(ncores)))
    return res


FUSED = True


def kernel(**inputs):
    x = np.asarray(inputs["x"], np.float32)
    positions = np.asarray(inputs["positions"])
    w = {k: np.asarray(v) for k, v in inputs.items() if k not in ("x", "positions")}
    if FUSED:
        res = run_layers(x, positions, w, list(range(DEPTH)))
        return np.stack([r["y"] for r in res.results], axis=0).astype(np.float32)
    cur = x
    for L in range(DEPTH):
        res = run_layers(cur, positions, w, [L])
        cur = np.stack([r["y"] for r in res.results], axis=0).astype(np.float32)
    return cur
```
